# Optimizing a Trainium2 kernel written in Bass

```python
import jax, jax.numpy as jnp
from jax import lax
import numpy as np

D_MODEL = 1024
BATCH = 16
SEQ = 4096
DEPTH = 1

EPS = 1e-6
NSA_HEADS = 8
NSA_KV_GROUPS = 2
NSA_REP = NSA_HEADS // NSA_KV_GROUPS
HEAD_DIM = 64
NSA_WIDTH = NSA_HEADS * HEAD_DIM
KV_WIDTH = NSA_KV_GROUPS * HEAD_DIM
CMP_STRIDE = 16
CMP_BLOCK = 2 * CMP_STRIDE
CMP_HIDDEN = 256
SEL_BLOCK = 64
SEL_TOP_N = 16
SEL_Q_BLOCK = 64
WINDOW = 512
WIN_Q_BLOCK = 128
ROPE_DIM = HEAD_DIM // 4
ROPE_THETA = 500000.0
SSD_HEADS = 8
SSD_HEAD_DIM = 64
SSD_WIDTH = SSD_HEADS * SSD_HEAD_DIM
SSD_GROUPS = 2
SSD_REP = SSD_HEADS // SSD_GROUPS
SSD_STATE = 128
SSD_CONV = 4
SSD_CHUNK = 128
SSD_CONV_DIM = SSD_WIDTH + 2 * SSD_GROUPS * SSD_STATE
MIX_WIDTH = NSA_WIDTH + SSD_WIDTH
IN_PROJ = NSA_WIDTH + 6 * KV_WIDTH + 3 * NSA_HEADS + SSD_WIDTH + SSD_CONV_DIM + SSD_HEADS
PEER_HEADS = 8
PEER_N_KEYS = 128
PEER_N_EXPERTS = PEER_N_KEYS * PEER_N_KEYS
PEER_KEY_DIM = 256
PEER_HALF = PEER_KEY_DIM // 2
PEER_TOPK = 16
PEER_TOK_BLOCK = 128

kernel_name = "hymba_nsa_ssd_peer_layer"


def rmsnorm(x, g):
    xf = x.astype(jnp.float32)
    y = xf * lax.rsqrt(jnp.mean(xf * xf, axis=-1, keepdims=True) + EPS)
    return (y * g.astype(jnp.float32)).astype(x.dtype)


def masked_softmax(s, mask):
    s = jnp.where(mask, s.astype(jnp.float32), -1e30)
    return jax.nn.softmax(s, axis=-1) * mask


def partial_rope(x, pos):
    half = ROPE_DIM // 2
    inv = jnp.power(ROPE_THETA, -jnp.arange(half, dtype=jnp.float32) * 2.0 / ROPE_DIM)
    ang = pos.astype(jnp.float32)[:, None] * inv[None, :]
    cos, sin = jnp.cos(ang).astype(x.dtype), jnp.sin(ang).astype(x.dtype)
    x1, x2, xp = x[..., :half], x[..., half:ROPE_DIM], x[..., ROPE_DIM:]
    return jnp.concatenate([x1 * cos - x2 * sin, x2 * cos + x1 * sin, xp], axis=-1)


def in_proj_splits():
    widths = [NSA_WIDTH] + [KV_WIDTH] * 6 + [3 * NSA_HEADS, SSD_WIDTH, SSD_CONV_DIM, SSD_HEADS]
    return [int(v) for v in np.cumsum(widths)[:-1]]


def compress(kv, pos_emb, w1, b1, w2):
    b, g, s, d = kv.shape
    chunks = kv.reshape(b, g, s // CMP_STRIDE, CMP_STRIDE, d)
    blocks = jnp.concatenate([chunks[:, :, :-1], chunks[:, :, 1:]], axis=3)
    blocks = (blocks + pos_emb).reshape(b, g, -1, CMP_BLOCK * d)
    return jax.nn.gelu(blocks @ w1 + b1) @ w2


def nsa_mixer(q, k_c, v_c, k_s, v_s, k_w, v_w, gates, cmp_k, cmp_v):
    b, g, r, s, d = q.shape
    scale = d ** -0.5
    t = jnp.arange(s)
    kc = compress(k_c, *cmp_k)
    vc = compress(v_c, *cmp_v)
    n_cmp = s // CMP_STRIDE - 1
    cmp_end = jnp.arange(n_cmp) * CMP_STRIDE + CMP_BLOCK - 1
    mask_c = cmp_end[None, :] <= t[:, None]
    p_cmp = masked_softmax(jnp.einsum('bgrtd,bgjd->bgrtj', q, kc) * scale, mask_c)
    o_cmp = jnp.einsum('bgrtj,bgjd->bgrtd', p_cmp.astype(vc.dtype), vc)
    n_sel = s // SEL_BLOCK
    n_top = min(SEL_TOP_N, n_sel)
    cs = np.arange(n_cmp) * CMP_STRIDE
    ss = np.arange(n_sel) * SEL_BLOCK
    overlap = (cs[:, None] < ss[None, :] + SEL_BLOCK) & (cs[:, None] + CMP_BLOCK > ss[None, :])
    overlap = jnp.asarray(overlap, jnp.float32)
    imp = jnp.einsum('bgrtj,ji->bgti', p_cmp, overlap)
    blk = jnp.arange(n_sel)
    cur = t // SEL_BLOCK
    forced = (blk[None, :] == 0) | (blk[None, :] == cur[:, None]) | (blk[None, :] == cur[:, None] - 1)
    valid = blk[None, :] <= cur[:, None]
    imp = jnp.where(forced, 1e9, jnp.where(valid, imp, -1.0))
    _, sel_idx = lax.top_k(imp, n_top)
    k_blk = k_s.reshape(b, g, n_sel, SEL_BLOCK, d)
    v_blk = v_s.reshape(b, g, n_sel, SEL_BLOCK, d)
    nqb = s // SEL_Q_BLOCK
    q_b = jnp.moveaxis(q.reshape(b, g, r, nqb, SEL_Q_BLOCK, d), 3, 0)
    idx_b = jnp.moveaxis(sel_idx.reshape(b, g, nqb, SEL_Q_BLOCK, n_top), 2, 0)
    t_b = t.reshape(nqb, SEL_Q_BLOCK)
    bi = jnp.arange(b)[:, None, None, None]
    gi = jnp.arange(g)[None, :, None, None]

    def sel_block(args):
        qb, ib, tb = args
        kg = k_blk[bi, gi, ib].reshape(b, g, SEL_Q_BLOCK, n_top * SEL_BLOCK, d)
        vg = v_blk[bi, gi, ib].reshape(b, g, SEL_Q_BLOCK, n_top * SEL_BLOCK, d)
        kpos = (ib[..., None] * SEL_BLOCK + jnp.arange(SEL_BLOCK)).reshape(b, g, SEL_Q_BLOCK, -1)
        mask = (kpos <= tb[:, None])[:, :, None]
        p = masked_softmax(jnp.einsum('bgrqd,bgqkd->bgrqk', qb, kg) * scale, mask)
        return jnp.einsum('bgrqk,bgqkd->bgrqd', p.astype(vg.dtype), vg)

    o_sel = lax.map(sel_block, (q_b, idx_b, t_b))
    o_sel = jnp.moveaxis(o_sel, 0, 3).reshape(b, g, r, s, d)
    nwb = s // WIN_Q_BLOCK
    n_ctx = WINDOW // WIN_Q_BLOCK + 1

    def band(kv):
        kp = jnp.pad(kv, ((0, 0), (0, 0), (WINDOW, 0), (0, 0))).reshape(b, g, nwb + n_ctx - 1, WIN_Q_BLOCK, d)
        return jnp.concatenate([kp[:, :, j:j + nwb] for j in range(n_ctx)], axis=3)

    kwb, vwb = band(k_w), band(v_w)
    qw = q.reshape(b, g, r, nwb, WIN_Q_BLOCK, d)
    tq = t.reshape(nwb, WIN_Q_BLOCK)
    tk = (jnp.arange(nwb)[:, None] - (n_ctx - 1)) * WIN_Q_BLOCK + jnp.arange(n_ctx * WIN_Q_BLOCK)[None, :]
    diff = tq[:, :, None] - tk[:, None, :]
    mask_w = (tk[:, None, :] >= 0) & (diff >= 0) & (diff < WINDOW)
    p_w = masked_softmax(jnp.einsum('bgrnqd,bgnkd->bgrnqk', qw, kwb) * scale, mask_w)
    o_win = jnp.einsum('bgrnqk,bgnkd->bgrnqd', p_w.astype(vwb.dtype), vwb).reshape(b, g, r, s, d)
    gr = gates.reshape(b, s, g, r, 3).transpose(0, 2, 3, 1, 4)
    o = gr[..., 0:1] * o_cmp + gr[..., 1:2] * o_sel + gr[..., 2:3] * o_win
    return o.transpose(0, 3, 1, 2, 4).reshape(b, s, g * r * d)


def ssd_mixer(z, xbc, dt, conv_w, conv_b, dt_bias, a_log, d_skip, norm_g):
    b, s, _ = xbc.shape
    xbc = lax.conv_general_dilated(xbc, conv_w[:, None, :], window_strides=(1,),
                                   padding=[(SSD_CONV - 1, 0)],
                                   dimension_numbers=('NWC', 'WIO', 'NWC'),
                                   feature_group_count=SSD_CONV_DIM) + conv_b
    xbc = jax.nn.silu(xbc)
    xs, bm, cm = jnp.split(xbc, [SSD_WIDTH, SSD_WIDTH + SSD_GROUPS * SSD_STATE], axis=-1)
    nc, l = s // SSD_CHUNK, SSD_CHUNK
    xs = xs.reshape(b, nc, l, SSD_GROUPS, SSD_REP, SSD_HEAD_DIM)
    bm = bm.reshape(b, nc, l, SSD_GROUPS, SSD_STATE).astype(jnp.float32)
    cm = cm.reshape(b, nc, l, SSD_GROUPS, SSD_STATE).astype(jnp.float32)
    dt = jax.nn.softplus(dt.astype(jnp.float32) + dt_bias.astype(jnp.float32))
    a = -jnp.exp(a_log.astype(jnp.float32))
    dt_c = dt.reshape(b, nc, l, SSD_GROUPS, SSD_REP)
    adt = (dt_c * a.reshape(SSD_GROUPS, SSD_REP)).transpose(0, 3, 4, 1, 2)
    xdt = xs.astype(jnp.float32) * dt_c[..., None]
    acum = jnp.cumsum(adt, axis=-1)
    causal = jnp.tril(jnp.ones((l, l), bool))
    seg = jnp.exp(jnp.where(causal, acum[..., :, None] - acum[..., None, :], -jnp.inf))
    cb = jnp.einsum('bclgn,bcsgn->bgcls', cm, bm)
    y_diag = jnp.einsum('bgcls,bgrcls,bcsgrp->bclgrp', cb, seg, xdt)
    decay_states = jnp.exp(acum[..., -1:] - acum)
    states = jnp.einsum('bclgn,bgrcl,bclgrp->cbgrpn', bm, decay_states, xdt)
    chunk_decay = jnp.moveaxis(jnp.exp(acum[..., -1]), -1, 0)

    def step(h, inp):
        st, dec = inp
        return h * dec[..., None, None] + st, h

    _, states_in = lax.scan(step, jnp.zeros(states.shape[1:], states.dtype), (states, chunk_decay))
    y_off = jnp.einsum('bclgn,cbgrpn,bgrcl->bclgrp', cm, states_in, jnp.exp(acum))
    y = y_diag + y_off + xs.astype(jnp.float32) * d_skip.astype(jnp.float32).reshape(SSD_GROUPS, SSD_REP, 1)
    y = y.reshape(b, s, SSD_WIDTH).astype(z.dtype)
    yg = (y * jax.nn.silu(z)).reshape(b, s, SSD_GROUPS, SSD_WIDTH // SSD_GROUPS)
    return rmsnorm(yg, norm_g.reshape(SSD_GROUPS, -1)).reshape(b, s, SSD_WIDTH)


def peer(x, w_q, sub_keys, w_u, w_v):
    b, s, dm = x.shape
    xt = x.reshape(b * s, dm)
    q = (xt @ w_q).reshape(-1, PEER_HEADS, 2, PEER_HALF)
    s1 = jnp.einsum('thd,hkd->thk', q[:, :, 0], sub_keys[:, 0])
    s2 = jnp.einsum('thd,hkd->thk', q[:, :, 1], sub_keys[:, 1])
    v1, i1 = lax.top_k(s1, PEER_TOPK)
    v2, i2 = lax.top_k(s2, PEER_TOPK)
    cand = (v1[..., :, None] + v2[..., None, :]).reshape(-1, PEER_HEADS, PEER_TOPK * PEER_TOPK)
    vals, ci = lax.top_k(cand, PEER_TOPK)
    e1 = jnp.take_along_axis(i1, ci // PEER_TOPK, axis=-1)
    e2 = jnp.take_along_axis(i2, ci % PEER_TOPK, axis=-1)
    idx = e1 * PEER_N_KEYS + e2
    gate = jax.nn.softmax(vals.astype(jnp.float32), axis=-1).astype(x.dtype)
    nb = xt.shape[0] // PEER_TOK_BLOCK

    def block(args):
        xb, ib, gb = args
        h = jax.nn.gelu(jnp.einsum('td,thkd->thk', xb, w_u[ib]))
        return jnp.einsum('thk,thkd->td', gb * h, w_v[ib])

    out = lax.map(block, (xt.reshape(nb, PEER_TOK_BLOCK, dm),
                          idx.reshape(nb, PEER_TOK_BLOCK, PEER_HEADS, PEER_TOPK),
                          gate.reshape(nb, PEER_TOK_BLOCK, PEER_HEADS, PEER_TOPK)))
    return out.reshape(b, s, dm)


def setup_inputs(seed: int = 0) -> dict:
    key = jax.random.key(seed)
    ks = jax.random.split(key, 32)
    n = jax.random.normal
    L = DEPTH
    dt0 = jnp.exp(jax.random.uniform(ks[14], (L, SSD_HEADS)) * (jnp.log(0.1) - jnp.log(0.001)) + jnp.log(0.001))
    return {
        "x": n(ks[0], (BATCH, SEQ, D_MODEL), jnp.float32),
        "attn_norm_g": 1.0 + 0.02 * n(ks[1], (L, D_MODEL)),
        "w_in": n(ks[2], (L, D_MODEL, IN_PROJ)) * D_MODEL ** -0.5,
        "cmp_pos_k": 0.02 * n(ks[3], (L, CMP_BLOCK, HEAD_DIM)),
        "cmp_w1_k": n(ks[4], (L, CMP_BLOCK * HEAD_DIM, CMP_HIDDEN)) * (CMP_BLOCK * HEAD_DIM) ** -0.5,
        "cmp_b1_k": 0.01 * n(ks[5], (L, CMP_HIDDEN)),
        "cmp_w2_k": n(ks[6], (L, CMP_HIDDEN, HEAD_DIM)) * CMP_HIDDEN ** -0.5,
        "cmp_pos_v": 0.02 * n(ks[7], (L, CMP_BLOCK, HEAD_DIM)),
        "cmp_w1_v": n(ks[8], (L, CMP_BLOCK * HEAD_DIM, CMP_HIDDEN)) * (CMP_BLOCK * HEAD_DIM) ** -0.5,
        "cmp_b1_v": 0.01 * n(ks[9], (L, CMP_HIDDEN)),
        "cmp_w2_v": n(ks[10], (L, CMP_HIDDEN, HEAD_DIM)) * CMP_HIDDEN ** -0.5,
        "conv_w": n(ks[11], (L, SSD_CONV, SSD_CONV_DIM)) * SSD_CONV ** -0.5,
        "conv_b": 0.01 * n(ks[12], (L, SSD_CONV_DIM)),
        "dt_bias": dt0 + jnp.log(-jnp.expm1(-dt0)),
        "a_log": jnp.log(jax.random.uniform(ks[13], (L, SSD_HEADS), minval=1.0, maxval=16.0)),
        "d_skip": 1.0 + 0.02 * n(ks[15], (L, SSD_HEADS)),
        "ssd_norm_g": 1.0 + 0.02 * n(ks[16], (L, SSD_WIDTH)),
        "nsa_norm_g": 1.0 + 0.02 * n(ks[17], (L, NSA_WIDTH)),
        "w_out": n(ks[18], (L, MIX_WIDTH, D_MODEL)) * MIX_WIDTH ** -0.5,
        "ffn_norm_g": 1.0 + 0.02 * n(ks[19], (L, D_MODEL)),
        "peer_w_q": n(ks[20], (L, D_MODEL, PEER_HEADS * PEER_KEY_DIM)) * D_MODEL ** -0.5,
        "peer_keys": n(ks[21], (L, PEER_HEADS, 2, PEER_N_KEYS, PEER_HALF)) * PEER_HALF ** -0.5,
        "peer_u": n(ks[22], (L, PEER_N_EXPERTS, D_MODEL)) * D_MODEL ** -0.5,
        "peer_v": n(ks[23], (L, PEER_N_EXPERTS, D_MODEL)) * PEER_HEADS ** -0.5,
        "final_norm_g": 1.0 + 0.02 * n(ks[24], (D_MODEL,)),
    }


def reference(x, attn_norm_g, w_in, cmp_pos_k, cmp_w1_k, cmp_b1_k, cmp_w2_k,
              cmp_pos_v, cmp_w1_v, cmp_b1_v, cmp_w2_v, conv_w, conv_b, dt_bias, a_log,
              d_skip, ssd_norm_g, nsa_norm_g, w_out, ffn_norm_g, peer_w_q, peer_keys,
              peer_u, peer_v, final_norm_g):
    b, s, _ = x.shape
    pos = jnp.arange(s)
    splits = in_proj_splits()
    for layer in range(DEPTH):
        h = rmsnorm(x, attn_norm_g[layer])
        proj = h @ w_in[layer]
        q, kc, vc, ksl, vsl, kw, vw, gl, z, xbc, dt = jnp.split(proj, splits, axis=-1)
        qh = partial_rope(q.reshape(b, s, NSA_KV_GROUPS, NSA_REP, HEAD_DIM).transpose(0, 2, 3, 1, 4), pos)

        def kv_heads(t_, rope):
            t_ = t_.reshape(b, s, NSA_KV_GROUPS, HEAD_DIM).transpose(0, 2, 1, 3)
            return partial_rope(t_, pos) if rope else t_

        gates = jax.nn.sigmoid(gl.astype(jnp.float32)).astype(x.dtype).reshape(b, s, NSA_HEADS, 3)
        o_nsa = nsa_mixer(qh, kv_heads(kc, True), kv_heads(vc, False), kv_heads(ksl, True),
                          kv_heads(vsl, False), kv_heads(kw, True), kv_heads(vw, False), gates,
                          (cmp_pos_k[layer], cmp_w1_k[layer], cmp_b1_k[layer], cmp_w2_k[layer]),
                          (cmp_pos_v[layer], cmp_w1_v[layer], cmp_b1_v[layer], cmp_w2_v[layer]))
        o_nsa = rmsnorm(o_nsa, nsa_norm_g[layer])
        o_ssd = ssd_mixer(z, xbc, dt, conv_w[layer], conv_b[layer], dt_bias[layer], a_log[layer],
                          d_skip[layer], ssd_norm_g[layer])
        x = x + jnp.concatenate([o_nsa, o_ssd.astype(o_nsa.dtype)], axis=-1) @ w_out[layer]
        x = x + peer(rmsnorm(x, ffn_norm_g[layer]), peer_w_q[layer], peer_keys[layer],
                     peer_u[layer], peer_v[layer])
    return rmsnorm(x, final_norm_g)
```

```python
from contextlib import ExitStack
import numpy as np
import ml_dtypes
import concourse.bass as bass
import concourse.mybir as mybir
from concourse.bass_utils import run_bass_kernel_spmd

F32 = mybir.dt.float32
BF16 = mybir.dt.bfloat16
I32 = mybir.dt.int32
U32 = mybir.dt.uint32
ALU = mybir.AluOpType
AF = mybir.ActivationFunctionType
AX = mybir.AxisListType

D = 1024
NH = 8
HD = 64
INP = 2848
EPS = 1e-6
NEG = -30000.0
POOL_SHARE = False


class Sem:
    def __init__(self, h, name):
        self.h = h
        self.cnt = 0
        self.name = name


class Buf:
    __slots__ = ("name", "w", "r")

    def __init__(self, name=""):
        self.name = name
        self.w = None
        self.r = {}


class Prog:
    EPOCH = 30000

    def __init__(self, nc, es):
        self.nc = nc
        self.es = es
        self.engs = ["pe", "act", "dve", "pool", "sp"]
        self.streams = {e: [] for e in self.engs}
        self.seq = {e: 0 for e in self.engs}
        self.esems = {e: [] for e in self.engs}
        self.known = {e: {} for e in self.engs}
        self.nsem = 0
        self.dsems = []
        self.phase_sems = {}
        self.free_sems = {}
        self.defer = None

    def newsem(self, name):
        self.nsem += 1
        h = self.es.enter_context(self.nc.semaphore(f"{name}_{self.nsem}"))
        return Sem(h, name)

    def dsem(self, name="d"):
        s = self.newsem(name)
        self.dsems.append(s)
        return s

    def S(self, key):
        return ("SEMKEY", key)

    def _resolve_sem(self, key, q):
        cls = "sw" if q == "pool" else "hw"
        k = (cls, key)
        if k not in self.phase_sems:
            fl = self.free_sems.setdefault(cls, [])
            sm = fl.pop() if fl else self.dsem("d" + cls)
            self.phase_sems[k] = sm
        return self.phase_sems[k]

    def end_phase(self):
        self.barrier()
        for (cls, _), sm in self.phase_sems.items():
            self.free_sems.setdefault(cls, []).append(sm)
        self.phase_sems = {}

    def _etok(self, e, k):
        idx = k // self.EPOCH
        while len(self.esems[e]) <= idx:
            self.esems[e].append(self.newsem(f"e{e}"))
        return (self.esems[e][idx], k % self.EPOCH + 1, e)

    def _waits(self, e, deps, skip_sem=None):
        need = {}
        for tok in deps:
            if tok is None:
                continue
            sem, val, src = tok
            if skip_sem is not None and sem is skip_sem:
                continue
            if e == "pe" and src == "pe":
                continue
            if self.known[e].get(sem, 0) >= val:
                continue
            if need.get(sem, 0) < val:
                need[sem] = val
        for sem, val in need.items():
            self.known[e][sem] = val
            self.streams[e].append(("wait", sem, val))

    def _deps(self, reads, writes):
        deps = []
        for b in reads:
            deps.append(b.w)
        for b in writes:
            deps.append(b.w)
            for s, (v, src) in b.r.items():
                deps.append((s, v, src))
        return deps

    def _mark(self, tok, reads, writes):
        sem, val, src = tok
        for b in reads:
            b.r[sem] = (val, src)
        for b in writes:
            b.w = tok
            b.r = {}

    def op(self, e, fn, reads=(), writes=(), nodep=()):
        if self.defer is not None:
            self.defer.append(("op", e, fn, tuple(reads), tuple(writes), tuple(nodep)))
            return None
        self._waits(e, self._deps(reads, writes))
        k = self.seq[e]
        self.seq[e] += 1
        tok = self._etok(e, k)
        self.streams[e].append(("op", fn, tok[0], 1))
        self._mark(tok, reads, writes)
        for b in nodep:
            b.w = tok
        return tok

    def dma(self, q, fn, sem, reads=(), writes=()):
        if self.defer is not None:
            self.defer.append(("dma", q, fn, sem, tuple(reads), tuple(writes)))
            return None
        if isinstance(sem, tuple):
            sem = self._resolve_sem(sem[1], q)
        self._waits(q, self._deps(reads, writes), skip_sem=sem)
        sem.cnt += 16
        tok = (sem, sem.cnt, "dma")
        self.streams[q].append(("op", fn, sem, 16))
        self._mark(tok, reads, writes)
        return tok

    def replay(self, items):
        for it in items:
            if it[0] == "op":
                self.op(it[1], it[2], it[3], it[4], it[5])
            else:
                self.dma(it[1], it[2], it[3], it[4], it[5])

    def barrier(self):
        toks = []
        for e in self.engs:
            if self.seq[e] > 0:
                toks.append(self._etok(e, self.seq[e] - 1))
        for s in self.dsems:
            if s.cnt > 0:
                toks.append((s, s.cnt, "dma"))
        for e in self.engs:
            self._waits(e, [t for t in toks if not (t[2] == e)])
        for e in ("act", "dve", "pool"):
            if self.seq[e] > 0:
                self._waits(e, [self._etok(e, self.seq[e] - 1)])

    def emit(self):
        nc = self.nc
        streams = self.streams

        def run(eng, lst):
            for it in lst:
                if it[0] == "wait":
                    eng.wait_ge(it[1].h, it[2])
                else:
                    ins = it[1](eng)
                    ins.then_inc(it[2].h, it[3])

        with nc.Block() as block:
            @block.tensor
            def _(eng):
                run(eng, streams["pe"])

            @block.scalar
            def _(eng):
                run(eng, streams["act"])

            @block.vector
            def _(eng):
                run(eng, streams["dve"])

            @block.gpsimd
            def _(eng):
                run(eng, streams["pool"])

            @block.sync
            def _(eng):
                run(eng, streams["sp"])


def _wcols():
    o = {}
    c = 0
    for n, w in [("q", 512), ("kc", 128), ("vc", 128), ("ks", 128), ("vs", 128), ("kw", 128),
                 ("vw", 128), ("gl", 24), ("z", 512), ("xbc", 1024), ("dt", 8)]:
        o[n] = (c, c + w)
        c += w
    return o


WC = _wcols()
W_ORDER = ["q", "kc", "ks", "kw", "vc", "vs", "vw", "gl", "dt", "z", "xbc"]


class Tile:
    def __init__(self, h, name):
        self.h = h
        self.b = Buf(name)

    def __getitem__(self, k):
        return self.h[k]


class View:
    def __init__(self, ap, b):
        self.ap = ap
        self.b = b

    def __getitem__(self, k):
        return self.ap[k]


class Ctx:
    pass


def build(NSEQ, S, phases="ABCDE", dbg=()):
    nc = bass.Bass("TRN2", target_bir_lowering=False)
    es = ExitStack()
    P = Prog(nc, es)
    C = Ctx()
    C.nc, C.es, C.P, C.NSEQ, C.S = nc, es, P, NSEQ, S
    T = NSEQ * S
    NTS = S // 128
    C.T, C.NTS = T, NTS
    C.dbg = dbg

    def din(name, shape, dt=F32):
        return nc.dram_tensor(name, list(shape), dt, kind="ExternalInput").ap()

    def dscr(name, shape, dt):
        kind = "ExternalOutput" if name in dbg else "Internal"
        return nc.dram_tensor(name, list(shape), dt, kind=kind).ap()

    def dout(name, shape, dt=F32):
        return nc.dram_tensor(name, list(shape), dt, kind="ExternalOutput").ap()

    C.din, C.dscr, C.dout = din, dscr, dout
    I = C.I = {}
    I["x"] = din("x", [T, D])
    I["attn_norm_g"] = din("attn_norm_g", [1, D])
    I["w_in"] = din("w_in", [D, INP])
    I["cos"] = din("cos", [128, NTS, 8])
    I["sin"] = din("sin", [128, NTS, 8])
    for kv in "kv":
        I["cmp_pos_" + kv] = din("cmp_pos_" + kv, [32, 64])
        I["cmp_w1_" + kv] = din("cmp_w1_" + kv, [2048, 256])
        I["cmp_b1_" + kv] = din("cmp_b1_" + kv, [256, 1])
        I["cmp_w2_" + kv] = din("cmp_w2_" + kv, [256, 64])
    I["conv_w"] = din("conv_w", [4, 1024])
    I["conv_b"] = din("conv_b", [1024, 1])
    I["dt_bias"] = din("dt_bias", [1, 8])
    I["a_log"] = din("a_log", [1, 8])
    I["d_skip"] = din("d_skip", [1, 8])
    I["ssd_norm_g"] = din("ssd_norm_g", [1, 512])
    I["nsa_norm_g"] = din("nsa_norm_g", [1, 512])
    I["w_out"] = din("w_out", [D, D])
    I["ffn_norm_g"] = din("ffn_norm_g", [1, D])
    I["peer_w_q"] = din("peer_w_q", [D, 2048])
    I["peer_keys"] = din("peer_keys", [16, 128, 128])
    I["peer_u"] = din("peer_u", [16384, D])
    I["peer_v"] = din("peer_v", [16384, D])
    I["final_norm_g"] = din("final_norm_g", [1, D])
    C.out = dout("out", [T, D])

    Sx = C.Sx = {}
    Sx["QT"] = dscr("QT", [NSEQ, NTS, 8, 64, 128], BF16)
    Sx["KT"] = dscr("KT", [4, NSEQ, 128, S], BF16)
    Sx["V"] = dscr("V", [NSEQ, S, 256], BF16)
    Sx["G"] = dscr("G", [T, 24], F32)
    Sx["DT"] = dscr("DT", [T, 8], F32)
    Sx["Z"] = dscr("Z", [T, 512], F32)
    Sx["XBC"] = dscr("XBC", [NSEQ, 1024, S], F32)
    Sx["MIX"] = dscr("MIX", [T, 1024], F32)
    C.dbufs = {}

    def db(*key):
        if key not in C.dbufs:
            C.dbufs[key] = Buf(str(key))
        return C.dbufs[key]

    C.db = db
    C.dbg_out = {}
    if "D" in phases:
        C.UV = dscr("peer_uv_bf", [16384, 2 * D], BF16)
        C.tab = Buf("uv")
        C.tabsems = [P.dsem("tab")] * 2
        for k2, nm in enumerate(("peer_u", "peer_v")):
            for c in range(16):
                P.dma("pool", lambda e, nm=nm, k2=k2, c=c: e.dma_start(out=C.UV[c * 1024:(c + 1) * 1024, k2 * D:(k2 + 1) * D], in_=I[nm][c * 1024:(c + 1) * 1024, :]),
                      C.tabsems[k2], writes=[C.tab])
    if "A" in phases:
        phase_a(C)
    if "B" in phases:
        phase_b(C)
    if "C" in phases:
        phase_c(C)
    if "D" in phases:
        phase_d(C)
    P.barrier()
    P.emit()
    es.close()
    return nc


def mk_tiles(C, es):
    nc = C.nc

    def sb(name, shape, dt=F32):
        return Tile(es.enter_context(nc.sbuf_tensor(name, list(shape), dt)), name)

    def ps(name, shape, dt=F32):
        return Tile(es.enter_context(nc.psum_tensor(name, list(shape), dt)), name)

    return sb, ps


def phase_a(C):
    nc, P, I, Sx, db = C.nc, C.P, C.I, C.Sx, C.db
    NSEQ, S, NTS = C.NSEQ, C.S, C.NTS
    es = ExitStack()
    sb, ps = mk_tiles(C, es)
    W = sb("a_W", [128, 8, INP], BF16)
    gbc = sb("a_gbc", [128, D])
    cos = sb("a_cos", [128, NTS, 8])
    sin = sb("a_sin", [128, NTS, 8])
    ident = sb("a_ident", [128, 128], BF16)
    identf = sb("a_identf", [128, 128])
    junk = sb("a_junk", [128, D])
    NB = 2
    xt = [sb(f"a_x{j}", [128, D]) for j in range(NB)]
    ssq = [sb(f"a_ssq{j}", [128, 1]) for j in range(NB)]
    rstd = [sb(f"a_rstd{j}", [128, 1]) for j in range(NB)]
    hb = [sb(f"a_h{j}", [128, D], BF16) for j in range(NB)]
    hT = [sb(f"a_hT{j}", [128, 8, 128], BF16) for j in range(NB)]
    R = [sb(f"a_R{j}", [128, 1024]) for j in range(NB)]
    RB = [sb(f"a_RB{j}", [128, 1024], BF16) for j in range(NB)]
    tmp = [[sb(f"a_t{j}_{k}", [128, 14, 8]) for k in range(4)] for j in range(NB)]
    qkt = [sb(f"a_qkt{j}", [128, 8, 128], BF16) for j in range(NB)]
    vst = [sb(f"a_vst{j}", [128, 256], BF16) for j in range(NB)]
    gst = [sb(f"a_gst{j}", [128, 24]) for j in range(NB)]
    dtst = [sb(f"a_dtst{j}", [128, 8]) for j in range(NB)]
    zst = [sb(f"a_zst{j}", [128, 512]) for j in range(NB)]
    xbst = [sb(f"a_xbst{j}", [128, 8, 128]) for j in range(NB)]
    pA = ps("a_pA", [128, 512])
    pB = ps("a_pB", [128, 512])
    pC = ps("a_pC", [128, 512])
    pD = ps("a_pD", [128, 512])
    pT = ps("a_pT", [128, 8, 128], BF16)
    pQ = ps("a_pQ", [128, 8, 128], BF16)
    pX = [ps(f"a_pX{j}", [128, 4, 128]) for j in range(2)]

    wv = I["w_in"].rearrange("(c p) n -> p c n", p=128)
    for c in range(8):
        P.dma("pool", lambda e, c=c: e.dma_start(out=W[:, c, :], in_=wv[:, c, :]), P.S("W"), writes=[W.b])
    P.dma("sp", lambda e: e.dma_start(out=gbc[:], in_=I["attn_norm_g"].partition_broadcast(128)), P.S("gbc"), writes=[gbc.b])
    P.dma("sp", lambda e: e.dma_start(out=cos[:], in_=I["cos"]), P.S("cos"), writes=[cos.b])
    P.dma("sp", lambda e: e.dma_start(out=sin[:], in_=I["sin"]), P.S("sin"), writes=[sin.b])
    make_ident(C, ident, identf)

    cA, cB, cC, cD, cE = 0, 512, 1024, 1312, 1824
    for ti in range(NSEQ * NTS):
        j = ti % NB
        seq, i = divmod(ti, NTS)
        r0 = ti * 128
        P.dma("sp", lambda e, j=j, r0=r0: e.dma_start(out=xt[j][:], in_=I["x"][r0:r0 + 128, :]), P.S(("x", j)), writes=[xt[j].b])
        P.op("act", lambda e, j=j: e.activation(out=junk[:], in_=xt[j][:], func=AF.Square, accum_out=ssq[j][:]),
             reads=[xt[j].b], writes=[ssq[j].b])
        P.op("act", lambda e, j=j: e.activation(out=rstd[j][:], in_=ssq[j][:], func=AF.Sqrt, scale=1.0 / D, bias=EPS),
             reads=[ssq[j].b], writes=[rstd[j].b])
        P.op("dve", lambda e, j=j: e.reciprocal(out=rstd[j][:], in_=rstd[j][:]), reads=[rstd[j].b], writes=[rstd[j].b])
        P.op("dve", lambda e, j=j: e.scalar_tensor_tensor(out=hb[j][:], in0=xt[j][:], scalar=rstd[j][:, 0:1], in1=gbc[:],
                                                           op0=ALU.mult, op1=ALU.mult),
             reads=[xt[j].b, rstd[j].b, gbc.b], writes=[hb[j].b])
        for c in range(8):
            P.op("pe", lambda e, j=j, c=c: e.transpose(out=pT[:, c, :], in_=hb[j][:, c * 128:(c + 1) * 128], identity=ident[:]),
                 reads=[hb[j].b, ident.b], writes=[pT.b])
        P.op("act", lambda e, j=j: e.activation(out=hT[j][:], in_=pT[:], func=AF.Copy), reads=[pT.b], writes=[hT[j].b])
        for (pt, c0, c1) in ((pA, cA, cB), (pB, cB, cC), (pC, cC, cD), (pD, cD, cE)):
            for c in range(8):
                P.op("pe", lambda e, j=j, c=c, pt=pt, c0=c0, c1=c1: e.matmul(
                    pt[:, 0:c1 - c0], lhsT=hT[j][:, c, :], rhs=W[:, c, c0:c1], start=(c == 0), stop=(c == 7)),
                    reads=[hT[j].b, W.b], writes=[pt.b])
        for cc in range(8):
            for c in range(8):
                P.op("pe", lambda e, j=j, c=c, cc=cc: e.matmul(
                    pX[cc // 4][:, cc % 4, :], lhsT=W[:, c, cE + cc * 128:cE + (cc + 1) * 128], rhs=hT[j][:, c, :],
                    start=(c == 0), stop=(c == 7)), reads=[hT[j].b, W.b], writes=[pX[cc // 4].b])
        P.op("act", lambda e, j=j: e.activation(out=R[j][:, 0:512], in_=pA[:], func=AF.Copy, scale=0.125),
             reads=[pA.b], writes=[R[j].b])
        P.op("dve", lambda e, j=j: e.tensor_copy(out=R[j][:, 512:1024], in_=pB[:]), reads=[pB.b, R[j].b], writes=[R[j].b])
        P.op("act", lambda e, j=j: e.activation(out=RB[j][:], in_=R[j][:], func=AF.Copy), reads=[R[j].b], writes=[RB[j].b])
        R3 = R[j][:, 0:896].rearrange("p (h d) -> p h d", d=64)
        RB3 = RB[j][:, 0:896].rearrange("p (h d) -> p h d", d=64)
        cb_ = cos[:, i, :].unsqueeze(1).to_broadcast([128, 14, 8])
        sb_ = sin[:, i, :].unsqueeze(1).to_broadcast([128, 14, 8])
        t0, t1, t2, t3 = tmp[j]
        x1, x2 = R3[:, :, 0:8], R3[:, :, 8:16]
        P.op("dve", lambda e, t0=t0, x1=x1, cb_=cb_: e.tensor_mul(out=t0[:], in0=x1, in1=cb_), reads=[R[j].b, cos.b], writes=[t0.b])
        P.op("dve", lambda e, t1=t1, x2=x2, sb_=sb_: e.tensor_mul(out=t1[:], in0=x2, in1=sb_), reads=[R[j].b, sin.b], writes=[t1.b])
        P.op("dve", lambda e, t2=t2, x2=x2, cb_=cb_: e.tensor_mul(out=t2[:], in0=x2, in1=cb_), reads=[R[j].b, cos.b], writes=[t2.b])
        P.op("dve", lambda e, t3=t3, x1=x1, sb_=sb_: e.tensor_mul(out=t3[:], in0=x1, in1=sb_), reads=[R[j].b, sin.b], writes=[t3.b])
        P.op("dve", lambda e, RB3=RB3, t0=t0, t1=t1: e.tensor_sub(out=RB3[:, :, 0:8], in0=t0[:], in1=t1[:]),
             reads=[t0.b, t1.b, RB[j].b], writes=[RB[j].b])
        P.op("dve", lambda e, RB3=RB3, t2=t2, t3=t3: e.tensor_add(out=RB3[:, :, 8:16], in0=t2[:], in1=t3[:]),
             reads=[t2.b, t3.b, RB[j].b], writes=[RB[j].b])
        for m in range(8):
            P.op("pe", lambda e, j=j, m=m: e.transpose(out=pQ[:, m, :], in_=RB[j][:, m * 128:(m + 1) * 128], identity=ident[:]),
                 reads=[RB[j].b, ident.b], writes=[pQ.b])
        P.op("dve", lambda e, j=j: e.tensor_copy(out=qkt[j][:], in_=pQ[:]), reads=[pQ.b], writes=[qkt[j].b])
        P.op("act", lambda e, j=j: e.activation(out=vst[j][:], in_=pC[:, 0:256], func=AF.Copy), reads=[pC.b], writes=[vst[j].b])
        P.op("act", lambda e, j=j: e.activation(out=gst[j][:], in_=pC[:, 256:280], func=AF.Sigmoid), reads=[pC.b], writes=[gst[j].b])
        P.op("act", lambda e, j=j: e.activation(out=dtst[j][:], in_=pC[:, 280:288], func=AF.Copy), reads=[pC.b], writes=[dtst[j].b])
        P.op("act", lambda e, j=j: e.activation(out=zst[j][:], in_=pD[:], func=AF.Silu), reads=[pD.b], writes=[zst[j].b])
        P.op("dve", lambda e, j=j: e.tensor_copy(out=xbst[j][:, 0:4, :], in_=pX[0][:]), reads=[pX[0].b], writes=[xbst[j].b])
        P.op("dve", lambda e, j=j: e.tensor_copy(out=xbst[j][:, 4:8, :], in_=pX[1][:]), reads=[pX[1].b, xbst[j].b], writes=[xbst[j].b])
        for half in range(2):
            dst = Sx["QT"][seq, i, half::2, :, :].rearrange("m d t -> d m t")
            src = qkt[j][half * 64:(half + 1) * 64, 0:4, :]
            P.dma("pool", lambda e, dst=dst, src=src: e.dma_start(out=dst, in_=src), P.S(("qt", j, half)), reads=[qkt[j].b], writes=[db("QT", seq, i)])
        dst = Sx["KT"][:, seq, :, i * 128:(i + 1) * 128].rearrange("k p t -> p k t")
        P.dma("pool", lambda e, dst=dst, j=j: e.dma_start(out=dst, in_=qkt[j][:, 4:8, :]), P.S(("kt", j)), reads=[qkt[j].b], writes=[db("KT", seq, i)])
        P.dma("pool", lambda e, j=j, seq=seq, i=i: e.dma_start(out=Sx["V"][seq, i * 128:(i + 1) * 128, :], in_=vst[j][:]), P.S(("v", j)),
              reads=[vst[j].b], writes=[db("V", seq, i)])
        P.dma("pool", lambda e, j=j, r0=r0: e.dma_start(out=Sx["G"][r0:r0 + 128, :], in_=gst[j][:]), P.S(("g", j)), reads=[gst[j].b], writes=[db("G", ti)])
        P.dma("pool", lambda e, j=j, r0=r0: e.dma_start(out=Sx["DT"][r0:r0 + 128, :], in_=dtst[j][:]), P.S(("dt", j)), reads=[dtst[j].b], writes=[db("DT", ti)])
        P.dma("pool", lambda e, j=j, r0=r0: e.dma_start(out=Sx["Z"][r0:r0 + 128, :], in_=zst[j][:]), P.S(("z", j)), reads=[zst[j].b], writes=[db("Z", ti)])
        dst = Sx["XBC"][seq, :, i * 128:(i + 1) * 128].rearrange("(c p) t -> p c t", p=128)
        P.dma("pool", lambda e, dst=dst, j=j: e.dma_start(out=dst, in_=xbst[j][:]), P.S(("xbc", j)), reads=[xbst[j].b], writes=[db("XBC", seq, i)])
    P.end_phase()
    es.close()


def make_ident(C, ident, identf):
    nc, P = C.nc, C.P
    P.op("pool", lambda e: e.memset(identf[:], 1.0), writes=[identf.b])
    P.op("pool", lambda e: e.affine_select(out=identf[:], in_=identf[:], pattern=[[1, 128]], compare_op=ALU.is_equal,
                                            fill=0.0, base=0, channel_multiplier=-1), reads=[identf.b], writes=[identf.b])
    P.op("dve", lambda e: e.tensor_copy(out=ident[:], in_=identf[:]), reads=[identf.b], writes=[ident.b])


def host_maps(inputs, NSEQ, S, ncores):
    f = lambda a: np.ascontiguousarray(np.asarray(a, dtype=np.float32))
    x = f(inputs["x"])
    NTS = S // 128
    w_in = f(inputs["w_in"])[0]
    w_re = np.concatenate([w_in[:, WC[n][0]:WC[n][1]] for n in W_ORDER], axis=1)
    pos = (np.arange(NTS)[None, :] * 128 + np.arange(128)[:, None]).astype(np.float32)
    inv = np.power(np.float32(500000.0), -np.arange(8, dtype=np.float32) * 2.0 / 16).astype(np.float32)
    ang = pos[:, :, None] * inv[None, None, :]
    shared = {
        "attn_norm_g": f(inputs["attn_norm_g"]).reshape(1, D),
        "w_in": np.ascontiguousarray(w_re),
        "cos": np.cos(ang).astype(np.float32), "sin": np.sin(ang).astype(np.float32),
        "conv_w": f(inputs["conv_w"])[0], "conv_b": f(inputs["conv_b"]).reshape(1024, 1),
        "dt_bias": f(inputs["dt_bias"]).reshape(1, 8), "a_log": f(inputs["a_log"]).reshape(1, 8),
        "d_skip": f(inputs["d_skip"]).reshape(1, 8),
        "ssd_norm_g": f(inputs["ssd_norm_g"]).reshape(1, 512), "nsa_norm_g": f(inputs["nsa_norm_g"]).reshape(1, 512),
        "w_out": f(inputs["w_out"])[0], "ffn_norm_g": f(inputs["ffn_norm_g"]).reshape(1, D),
        "peer_w_q": f(inputs["peer_w_q"])[0], "peer_keys": f(inputs["peer_keys"]).reshape(16, 128, 128),
        "peer_u": f(inputs["peer_u"])[0], "peer_v": f(inputs["peer_v"])[0],
        "final_norm_g": f(inputs["final_norm_g"]).reshape(1, D),
    }
    for kv in "kv":
        shared["cmp_pos_" + kv] = f(inputs["cmp_pos_" + kv])[0]
        shared["cmp_w1_" + kv] = f(inputs["cmp_w1_" + kv])[0]
        shared["cmp_b1_" + kv] = f(inputs["cmp_b1_" + kv]).reshape(256, 1)
        shared["cmp_w2_" + kv] = f(inputs["cmp_w2_" + kv])[0]
    maps = []
    for c in range(ncores):
        m = dict(shared)
        m["x"] = np.ascontiguousarray(x[c * NSEQ:(c + 1) * NSEQ].reshape(NSEQ * S, D))
        maps.append(m)
    return maps


_NC_CACHE = {}


def kernel(**inputs):
    x = np.asarray(inputs["x"])
    B, S, _ = x.shape
    ncores = 8
    NSEQ = B // ncores
    key = (NSEQ, S)
    if key not in _NC_CACHE:
        _NC_CACHE[key] = build(NSEQ, S)
    nc = _NC_CACHE[key]
    maps = host_maps(inputs, NSEQ, S, ncores)
    res = run_bass_kernel_spmd(nc, maps, core_ids=list(range(ncores)))
    out = np.concatenate([np.asarray(r["out"]).reshape(NSEQ, S, D) for r in res.results], axis=0)
    return out.astype(np.float32)


def phase_b(C):
    nc, P, I, Sx, db = C.nc, C.P, C.I, C.Sx, C.db
    NSEQ, S, NTS = C.NSEQ, C.S, C.NTS
    NCMP = S // 16 - 1
    NSEL = S // 64
    es = ExitStack()
    sb, ps = mk_tiles(C, es)
    Sx["ONSA"] = C.dscr("ONSA", [C.T, 512], F32)
    ident = sb("b_ident", [128, 128], BF16)
    identf = sb("b_identf", [128, 128])
    w1 = {kv: sb("b_w1" + kv, [64, 32, 256], BF16) for kv in "kv"}
    w2 = {kv: sb("b_w2" + kv, [128, 2, 64], BF16) for kv in "kv"}
    posT = {kv: sb("b_pos" + kv, [64, 32], BF16) for kv in "kv"}
    b1 = {kv: sb("b_b1" + kv, [128, 2]) for kv in "kv"}
    b1t = {kv: sb("b_b1t" + kv, [128, 2]) for kv in "kv"}
    Kst = sb("b_Kst", [128, S], BF16)
    kwT = sb("b_kwT", [64, S], BF16)
    kcT = sb("b_kcT", [64, S], BF16)
    vcT = sb("b_vcT", [64, S], BF16)
    Vs = sb("b_Vs", [128, NTS, 65], BF16)
    Vw = sb("b_Vw", [128, NTS, 65], BF16)
    cm = sb("b_cm", [128, 8])
    cmaskT = sb("b_cmaskT", [128, 128], BF16)
    wmaskT = sb("b_wmaskT", [128, 128], BF16)
    A3 = sb("b_A3", [128, 3])
    M3 = sb("b_M3", [128, 3])
    zrow = sb("b_zrow", [1, 128], BF16)
    zrhs = sb("b_zrhs", [1, 512], BF16)
    hu = sb("b_hu", [128, 256])
    hw = sb("b_hw", [128, 256])
    hs = sb("b_hs", [128, 256])
    hidT = [sb(f"b_hidT{c}", [128, 256], BF16) for c in range(2)]
    kcmpT = sb("b_kcmpT", [64, 256], BF16)
    vcmp = sb("b_vcmp", [128, 2, 64], BF16)
    NB = 2
    Qst = [sb(f"b_Qst{j}", [128, 512], BF16) for j in range(NB)]
    gts = [sb(f"b_g{j}", [128, 24]) for j in range(NB)]
    sc = sb("b_sc", [128, 4, 256])
    pe_ = sb("b_pe", [128, 4, 256])
    pn = sb("b_pn", [128, 4, 256])
    zs = sb("b_zs", [128, 4])
    rz = sb("b_rz", [128, 4])
    PPW = 4 * NSEL + 4
    PP = sb("b_PP", [128, PPW])
    imp = sb("b_imp", [128, 64])
    imp2 = sb("b_imp2", [128, 64])
    m8a = sb("b_m8a", [128, 8])
    m8b = sb("b_m8b", [128, 8])
    biasm = sb("b_biasm", [128, 128])
    ptb = sb("b_ptb", [128, 2, 128], BF16)
    pts = [sb(f"b_pts{j}", [128, 512], BF16) for j in range(3)]
    ptw = [sb(f"b_ptw{j}", [128, 512], BF16) for j in range(3)]
    osb = sb("b_osb", [128, 4, 65])
    owb = sb("b_owb", [128, 4, 65])
    rsel = sb("b_rsel", [128, 4])
    rwin = sb("b_rwin", [128, 4])
    wsel = sb("b_wsel", [128, 4])
    wwin = sb("b_wwin", [128, 4])
    ot = [sb(f"b_ot{j}", [128, 4, 64]) for j in range(NB)]
    t2 = sb("b_t2", [128, 4, 64])
    pc = [ps(f"b_pc{j}", [128, 512]) for j in range(2)]
    pmisc = ps("b_pmisc", [128, 512])
    poc = ps("b_poc", [128, 4, 64])
    pss = [ps(f"b_pss{j}", [128, 512]) for j in range(2)]
    posel = ps("b_posel", [128, 512])
    powin = ps("b_powin", [128, 512])

    make_ident(C, ident, identf)
    for kv in "kv":
        w1v = I["cmp_w1_" + kv].rearrange("(p d) h -> d p h", d=64)
        for q4 in range(4):
            P.dma("pool", lambda e, kv=kv, q4=q4, w1v=w1v: e.dma_start(out=w1[kv][:, q4 * 8:(q4 + 1) * 8, :], in_=w1v[:, q4 * 8:(q4 + 1) * 8, :]),
                  P.S(("w1", kv)), writes=[w1[kv].b])
        P.dma("pool", lambda e, kv=kv: e.dma_start(out=w2[kv][:], in_=I["cmp_w2_" + kv].rearrange("(c p) d -> p c d", p=128)),
              P.S(("w2", kv)), writes=[w2[kv].b])
        P.dma("pool", lambda e, kv=kv: e.dma_start(out=posT[kv][:], in_=I["cmp_pos_" + kv].rearrange("p d -> d p"),
                                                    allow_slow_non_contiguous=True), P.S(("pos", kv)), writes=[posT[kv].b])
        P.dma("sp", lambda e, kv=kv: e.dma_start(out=b1[kv][:], in_=I["cmp_b1_" + kv].rearrange("(c p) o -> p (c o)", p=128),
                                                  allow_slow_non_contiguous=True), P.S(("b1", kv)), writes=[b1[kv].b])
        for hc in range(2):
            for p in range(32):
                P.op("pe", lambda e, kv=kv, hc=hc, p=p: e.matmul(pmisc[:, hc:hc + 1], lhsT=w1[kv][:, p, hc * 128:(hc + 1) * 128],
                                                                  rhs=posT[kv][:, p:p + 1], start=(p == 0), stop=(p == 31)),
                     reads=[w1[kv].b, posT[kv].b], writes=[pmisc.b])
        P.op("dve", lambda e, kv=kv: e.tensor_add(out=b1t[kv][:], in0=pmisc[:, 0:2], in1=b1[kv][:]),
             reads=[pmisc.b, b1[kv].b], writes=[b1t[kv].b])
    P.op("pool", lambda e: e.memset(Kst[64:128, :], 1.0), writes=[Kst.b])
    P.op("pool", lambda e: e.affine_select(out=Kst[64:128, :], in_=Kst[64:128, :], pattern=[[1, S]], compare_op=ALU.is_ge,
                                            fill=0.0, base=0, channel_multiplier=-64), reads=[Kst.b], writes=[Kst.b])
    P.op("pool", lambda e: e.affine_select(out=Kst[64:128, :], in_=Kst[64:128, :], pattern=[[-1, S]], compare_op=ALU.is_ge,
                                            fill=0.0, base=63, channel_multiplier=64), reads=[Kst.b], writes=[Kst.b])
    P.op("pool", lambda e: e.memset(cm[:], 0.0), writes=[cm.b])
    P.op("pool", lambda e: e.affine_select(out=cm[:], in_=cm[:], pattern=[[-16, 8]], compare_op=ALU.is_ge,
                                            fill=NEG, base=-15, channel_multiplier=1), reads=[cm.b], writes=[cm.b])
    P.op("pool", lambda e: e.memset(cmaskT[:], 1.0), writes=[cmaskT.b])
    P.op("pool", lambda e: e.affine_select(out=cmaskT[:], in_=cmaskT[:], pattern=[[1, 128]], compare_op=ALU.is_ge,
                                            fill=0.0, base=0, channel_multiplier=-1), reads=[cmaskT.b], writes=[cmaskT.b])
    P.op("pool", lambda e: e.memset(wmaskT[:], 1.0), writes=[wmaskT.b])
    P.op("pool", lambda e: e.affine_select(out=wmaskT[:], in_=wmaskT[:], pattern=[[-1, 128]], compare_op=ALU.is_gt,
                                            fill=0.0, base=0, channel_multiplier=1), reads=[wmaskT.b], writes=[wmaskT.b])
    P.op("dve", lambda e: e.memset(A3[:], 1e9), writes=[A3.b])
    P.op("dve", lambda e: e.memset(A3[64:128, 0:1], 0.0), reads=[A3.b], writes=[A3.b])
    P.op("dve", lambda e: e.memset(A3[0:64, 2:3], -1.0), reads=[A3.b], writes=[A3.b])
    P.op("dve", lambda e: e.memset(M3[:], 0.0), writes=[M3.b])
    P.op("dve", lambda e: e.memset(M3[64:128, 0:1], 1.0), reads=[M3.b], writes=[M3.b])
    P.op("dve", lambda e: e.memset(zrow[:], 0.0), writes=[zrow.b])
    P.op("dve", lambda e: e.memset(zrhs[:], 0.0), writes=[zrhs.b])
    P.op("dve", lambda e: e.memset(biasm[:], 0.0), writes=[biasm.b])
    P.op("dve", lambda e: e.memset(Vs[:], 1.0), writes=[Vs.b])
    P.op("dve", lambda e: e.memset(Vw[:], 1.0), writes=[Vw.b])

    cnt = 0
    for seq in range(NSEQ):
        for g in range(2):
            kbufs = [db("KT", seq, i) for i in range(NTS)]
            vbufs = [db("V", seq, i) for i in range(NTS)]
            rows = slice(g * 64, (g + 1) * 64)
            P.dma("sp", lambda e, seq=seq, rows=rows: e.dma_start(out=kcT[:], in_=Sx["KT"][0, seq, rows, :]), P.S("kc"), reads=kbufs, writes=[kcT.b])
            P.dma("sp", lambda e, seq=seq, rows=rows: e.dma_start(out=Kst[0:64, :], in_=Sx["KT"][1, seq, rows, :]), P.S("ks"), reads=kbufs, writes=[Kst.b])
            P.dma("sp", lambda e, seq=seq, rows=rows: e.dma_start(out=kwT[:], in_=Sx["KT"][2, seq, rows, :]), P.S("kw"), reads=kbufs, writes=[kwT.b])
            P.dma("sp", lambda e, seq=seq, rows=rows: e.dma_start(out=vcT[:], in_=Sx["KT"][3, seq, rows, :]), P.S("vc"), reads=kbufs, writes=[vcT.b])
            P.dma("sp", lambda e, seq=seq, g=g: e.dma_start(out=Vs[:, :, 0:64], in_=Sx["V"][seq, :, g * 64:(g + 1) * 64].rearrange("(i p) d -> p i d", p=128)),
                  P.S("vs"), reads=vbufs, writes=[Vs.b])
            P.dma("sp", lambda e, seq=seq, g=g: e.dma_start(out=Vw[:, :, 0:64], in_=Sx["V"][seq, :, 128 + g * 64:128 + (g + 1) * 64].rearrange("(i p) d -> p i d", p=128)),
                  P.S("vw"), reads=vbufs, writes=[Vw.b])
            for kv, src in (("k", kcT), ("v", vcT)):
                for hc in range(2):
                    hp = pss[hc]
                    for p in range(32):
                        P.op("pe", lambda e, kv=kv, hc=hc, p=p, src=src, hp=hp: e.matmul(
                            hp[:, 0:NCMP], lhsT=w1[kv][:, p, hc * 128:(hc + 1) * 128], rhs=src[:, p:p + 16 * (NCMP - 1) + 1:16],
                            start=(p == 0), stop=(p == 31)), reads=[w1[kv].b, src.b], writes=[hp.b])
                    P.op("act", lambda e, kv=kv, hc=hc, hp=hp: e.activation(out=hu[:, 0:NCMP], in_=hp[:, 0:NCMP], func=AF.Identity,
                                                                          bias=b1t[kv][:, hc:hc + 1]), reads=[hp.b, b1t[kv].b], writes=[hu.b])
                    P.op("dve", lambda e: e.tensor_mul(out=hw[:, 0:NCMP], in0=hu[:, 0:NCMP], in1=hu[:, 0:NCMP]), reads=[hu.b], writes=[hw.b])
                    P.op("dve", lambda e: e.tensor_scalar(out=hw[:, 0:NCMP], in0=hw[:, 0:NCMP], scalar1=0.044715, scalar2=1.0,
                                                          op0=ALU.mult, op1=ALU.add), reads=[hw.b], writes=[hw.b])
                    P.op("dve", lambda e: e.tensor_mul(out=hw[:, 0:NCMP], in0=hw[:, 0:NCMP], in1=hu[:, 0:NCMP]), reads=[hw.b, hu.b], writes=[hw.b])
                    P.op("act", lambda e: e.activation(out=hs[:, 0:NCMP], in_=hw[:, 0:NCMP], func=AF.Sigmoid, scale=1.5957691216057308),
                         reads=[hw.b], writes=[hs.b])
                    P.op("dve", lambda e, hc=hc: e.tensor_mul(out=hidT[hc][:, 0:NCMP], in0=hu[:, 0:NCMP], in1=hs[:, 0:NCMP]),
                         reads=[hu.b, hs.b], writes=[hidT[hc].b])
                if kv == "k":
                    for hc in range(2):
                        P.op("pe", lambda e, hc=hc: e.matmul(pc[0][0:64, 0:NCMP], lhsT=w2["k"][:, hc, :], rhs=hidT[hc][:, 0:NCMP],
                                                             start=(hc == 0), stop=(hc == 1)), reads=[w2["k"].b, hidT[hc].b], writes=[pc[0].b])
                    P.op("act", lambda e: e.activation(out=kcmpT[:, 0:NCMP], in_=pc[0][0:64, 0:NCMP], func=AF.Copy), reads=[pc[0].b], writes=[kcmpT.b])
                else:
                    for jc in range(2):
                        nj = min(128, NCMP - jc * 128)
                        if nj <= 0:
                            continue
                        for hc in range(2):
                            P.op("pe", lambda e, hc=hc, jc=jc, nj=nj: e.matmul(pc[1][0:nj, jc * 64:(jc + 1) * 64], lhsT=hidT[hc][:, jc * 128:jc * 128 + nj],
                                                                           rhs=w2["v"][:, hc, :], start=(hc == 0), stop=(hc == 1)),
                                 reads=[w2["v"].b, hidT[hc].b], writes=[pc[1].b])
                        P.op("act", lambda e, jc=jc, nj=nj: e.activation(out=vcmp[0:nj, jc, :], in_=pc[1][0:nj, jc * 64:(jc + 1) * 64], func=AF.Copy),
                             reads=[pc[1].b], writes=[vcmp.b])
            for i in range(NTS):
                j = cnt % NB
                cnt += 1
                ti = seq * NTS + i
                r0 = ti * 128
                Q = Qst[j]
                P.dma("sp", lambda e, Q=Q, seq=seq, i=i, g=g: e.dma_start(
                    out=Q[0:64, :].rearrange("d (r t) -> d r t", r=4), in_=Sx["QT"][seq, i, 4 * g:4 * g + 4, :, :].rearrange("r d t -> d r t")),
                    P.S(("q", j)), reads=[db("QT", seq, i)], writes=[Q.b])
                P.dma("sp", lambda e, j=j, r0=r0: e.dma_start(out=gts[j][:], in_=Sx["G"][r0:r0 + 128, :]), P.S(("gt", j)), reads=[db("G", ti)], writes=[gts[j].b])
                def branch(nm, pacc, ptl, lhs_of, rows_k, Vt, kts, kb, Q=Q, i=i):
                    P.op("pe", lambda e, pacc=pacc: e.matmul(pacc[:, 0:260], lhsT=zrow[:], rhs=zrhs[:, 0:260], start=True, stop=False, skip_group_check=True),
                         reads=[zrow.b, zrhs.b], writes=[pacc.b])
                    nk = len(kts)
                    for n_ in range(nk + 1):
                        if n_ < nk:
                            kt = kts[n_]
                            pb = pss[n_ % 2]
                            pt_ = ptl[n_ % 3]
                            P.op("pe", lambda e, pb=pb, kt=kt: e.matmul(pb[:], lhsT=lhs_of(kt), rhs=Q[0:rows_k, :], start=True, stop=True),
                                 reads=[kb, Q.b], writes=[pb.b])
                            P.op("act", lambda e, pb=pb, pt_=pt_: e.activation(out=pt_[:], in_=pb[:], func=AF.Exp), reads=[pb.b], writes=[pt_.b])
                            msk = None
                            if kt == i:
                                msk = cmaskT
                            elif nm == "w" and kt == i - 4:
                                msk = wmaskT
                            if msk is not None:
                                P.op("dve", lambda e, pt_=pt_, msk=msk: e.tensor_mul(out=pt_[:].rearrange("p (r t) -> p r t", r=4), in0=pt_[:].rearrange("p (r t) -> p r t", r=4),
                                                                                    in1=msk[:].unsqueeze(1).to_broadcast([128, 4, 128])),
                                     reads=[pt_.b, msk.b], writes=[pt_.b])
                        if n_ >= 1:
                            kt = kts[n_ - 1]
                            pt_ = ptl[(n_ - 1) % 3]
                            for r in range(4):
                                P.op("pe", lambda e, pacc=pacc, pt_=pt_, r=r, kt=kt, last=(n_ == nk): e.matmul(
                                    pacc[:, r * 65:(r + 1) * 65], lhsT=pt_[:, r * 128:(r + 1) * 128], rhs=Vt[:, kt, :], start=False, stop=last, skip_group_check=True),
                                    reads=[pt_.b, Vt.b], writes=[pacc.b])
                ncols = min(8 * i + 7, NCMP)
                for r in range(4):
                    P.op("pe", lambda e, Q=Q, r=r, ncols=ncols: e.matmul(pc[r // 2][:, (r % 2) * 256:(r % 2) * 256 + ncols], lhsT=Q[0:64, r * 128:(r + 1) * 128],
                                                                         rhs=kcmpT[:, 0:ncols], start=True, stop=True),
                         reads=[Q.b, kcmpT.b], writes=[pc[r // 2].b])
                for h2 in range(2):
                    P.op("act", lambda e, h2=h2, ncols=ncols: e.activation(out=sc[:, 2 * h2:2 * h2 + 2, 0:ncols],
                                                                          in_=pc[h2][:].rearrange("p (r c) -> p r c", r=2)[:, :, 0:ncols], func=AF.Copy),
                         reads=[pc[h2].b, sc.b], writes=[sc.b])
                lo = max(8 * i - 1, 0)
                hi = ncols
                a = lo - (8 * i - 1)
                P.op("dve", lambda e, lo=lo, hi=hi, a=a: e.tensor_add(out=sc[:, :, lo:hi], in0=sc[:, :, lo:hi],
                                                                     in1=cm[:, a:a + hi - lo].unsqueeze(1).to_broadcast([128, 4, hi - lo])),
                     reads=[sc.b, cm.b], writes=[sc.b])
                for r in range(4):
                    P.op("act", lambda e, r=r, ncols=ncols: e.activation(out=pe_[:, r, 0:ncols], in_=sc[:, r, 0:ncols], func=AF.Exp, accum_out=zs[:, r:r + 1]),
                         reads=[sc.b, pe_.b, zs.b], writes=[pe_.b, zs.b])
                P.op("dve", lambda e: e.tensor_scalar_add(out=rz[:], in0=zs[:], scalar1=1e-30), reads=[zs.b], writes=[rz.b])
                P.op("dve", lambda e: e.reciprocal(out=rz[:], in_=rz[:]), reads=[rz.b], writes=[rz.b])
                P.op("dve", lambda e, ncols=ncols: e.tensor_mul(out=pn[:, :, 0:ncols], in0=pe_[:, :, 0:ncols],
                                                                in1=rz[:].unsqueeze(2).to_broadcast([128, 4, ncols])), reads=[pe_.b, rz.b], writes=[pn.b])
                if 2 * i + 1 > 15:
                    P.op("dve", lambda e: e.memset(PP[:], 0.0), writes=[PP.b])
                    P.op("dve", lambda e, ncols=ncols: e.tensor_add(out=PP[:, 1:1 + ncols], in0=pn[:, 0, 0:ncols], in1=pn[:, 1, 0:ncols]), reads=[pn.b, PP.b], writes=[PP.b])
                    for r in (2, 3):
                        P.op("dve", lambda e, ncols=ncols, r=r: e.tensor_add(out=PP[:, 1:1 + ncols], in0=PP[:, 1:1 + ncols], in1=pn[:, r, 0:ncols]), reads=[pn.b, PP.b], writes=[PP.b])
                    P.op("dve", lambda e: e.tensor_reduce(out=imp[:, 0:NSEL], in_=PP[:, 0:4 * NSEL].rearrange("p (b m) -> p b m", m=4), axis=AX.X, op=ALU.add),
                         reads=[PP.b], writes=[imp.b])
                    P.op("dve", lambda e: e.tensor_add(out=imp[:, 0:NSEL], in0=imp[:, 0:NSEL], in1=PP[:, 4:4 * NSEL + 4:4]), reads=[PP.b, imp.b], writes=[imp.b])
                    if 2 * i + 2 < 64:
                        P.op("dve", lambda e, i=i: e.memset(imp[:, 2 * i + 2:64], -1.0), reads=[imp.b], writes=[imp.b])
                    P.op("dve", lambda e, i=i: e.tensor_mul(out=imp[:, 2 * i - 1:2 * i + 2], in0=imp[:, 2 * i - 1:2 * i + 2], in1=M3[:]), reads=[imp.b, M3.b], writes=[imp.b])
                    P.op("dve", lambda e, i=i: e.tensor_add(out=imp[:, 2 * i - 1:2 * i + 2], in0=imp[:, 2 * i - 1:2 * i + 2], in1=A3[:]), reads=[imp.b, A3.b], writes=[imp.b])
                    P.op("dve", lambda e: e.memset(imp[:, 0:1], 1e9), reads=[imp.b], writes=[imp.b])
                    P.op("dve", lambda e: e.max(out=m8a[:], in_=imp[:]), reads=[imp.b], writes=[m8a.b])
                    P.op("dve", lambda e: e.match_replace(out=imp2[:], in_to_replace=m8a[:], in_values=imp[:], imm_value=-2.0), reads=[imp.b, m8a.b], writes=[imp2.b])
                    P.op("dve", lambda e: e.max(out=m8b[:], in_=imp2[:]), reads=[imp2.b], writes=[m8b.b])
                    P.op("dve", lambda e: e.tensor_scalar(out=biasm[:, 64:128], in0=imp[:], scalar1=m8b[:, 7:8], scalar2=NEG, op0=ALU.is_lt, op1=ALU.mult),
                         reads=[imp.b, m8b.b, biasm.b], writes=[biasm.b])
                    pass
                branch("w", powin, ptw, lambda kt: kwT[:, kt * 128:(kt + 1) * 128], 64, Vw, list(range(max(0, i - 4), i + 1)), kwT.b)
                njc = (ncols + 127) // 128
                for r in range(4):
                    for jc in range(njc):
                        nj = min(128, ncols - jc * 128)
                        P.op("pe", lambda e, r=r, jc=jc, nj=nj: e.transpose(out=pmisc[0:nj, jc * 128:(jc + 1) * 128], in_=pn[:, r, jc * 128:jc * 128 + nj],
                                                                          identity=identf[:]), reads=[pn.b, identf.b], writes=[pmisc.b])
                        P.op("act", lambda e, jc=jc, nj=nj: e.activation(out=ptb[0:nj, jc, :], in_=pmisc[0:nj, jc * 128:(jc + 1) * 128], func=AF.Copy),
                             reads=[pmisc.b, ptb.b], writes=[ptb.b])
                    for jc in range(njc):
                        nj = min(128, ncols - jc * 128)
                        P.op("pe", lambda e, r=r, jc=jc, nj=nj, njc=njc: e.matmul(poc[:, r, :], lhsT=ptb[0:nj, jc, :], rhs=vcmp[0:nj, jc, :],
                                                                                 start=(jc == 0), stop=(jc == njc - 1)),
                             reads=[ptb.b, vcmp.b], writes=[poc.b])
                if 2 * i + 1 > 15:
                    P.op("pe", lambda e: e.transpose(out=pmisc[:, 256:384], in_=biasm[:], identity=identf[:]), reads=[biasm.b, identf.b], writes=[pmisc.b])
                    P.op("act", lambda e, Q=Q: e.activation(out=Q[64:128, :].rearrange("p (r t) -> p r t", r=4),
                                                           in_=pmisc[64:128, 256:384].unsqueeze(1).to_broadcast([64, 4, 128]), func=AF.Copy),
                         reads=[pmisc.b, Q.b], writes=[Q.b])
                else:
                    P.op("dve", lambda e, Q=Q: e.memset(Q[64:128, :], 0.0), reads=[Q.b], writes=[Q.b])
                branch("s", posel, pts, lambda kt: Kst[:, kt * 128:(kt + 1) * 128], 128, Vs, list(range(0, i + 1)), Kst.b)
                gv = gts[j][:, g * 12:(g + 1) * 12].rearrange("p (r k) -> p r k", k=3)
                P.op("act", lambda e: e.activation(out=osb[:], in_=posel[:, 0:260].rearrange("p (r c) -> p r c", c=65), func=AF.Copy), reads=[posel.b], writes=[osb.b])
                P.op("act", lambda e: e.activation(out=owb[:], in_=powin[:, 0:260].rearrange("p (r c) -> p r c", c=65), func=AF.Copy), reads=[powin.b], writes=[owb.b])
                P.op("dve", lambda e: e.reciprocal(out=rsel[:], in_=osb[:, :, 64]), reads=[osb.b], writes=[rsel.b])
                P.op("dve", lambda e: e.reciprocal(out=rwin[:], in_=owb[:, :, 64]), reads=[owb.b], writes=[rwin.b])
                P.op("dve", lambda e, gv=gv: e.tensor_mul(out=wsel[:], in0=rsel[:], in1=gv[:, :, 1]), reads=[rsel.b, gts[j].b], writes=[wsel.b])
                P.op("dve", lambda e, gv=gv: e.tensor_mul(out=wwin[:], in0=rwin[:], in1=gv[:, :, 2]), reads=[rwin.b, gts[j].b], writes=[wwin.b])
                o = ot[j]
                P.op("dve", lambda e, o=o, gv=gv: e.tensor_mul(out=o[:], in0=poc[:], in1=gv[:, :, 0:1].to_broadcast([128, 4, 64])), reads=[poc.b, gts[j].b], writes=[o.b])
                P.op("dve", lambda e: e.tensor_mul(out=t2[:], in0=osb[:, :, 0:64], in1=wsel[:].unsqueeze(2).to_broadcast([128, 4, 64])), reads=[osb.b, wsel.b], writes=[t2.b])
                P.op("dve", lambda e, o=o: e.tensor_add(out=o[:], in0=o[:], in1=t2[:]), reads=[o.b, t2.b], writes=[o.b])
                P.op("dve", lambda e: e.tensor_mul(out=t2[:], in0=owb[:, :, 0:64], in1=wwin[:].unsqueeze(2).to_broadcast([128, 4, 64])), reads=[owb.b, wwin.b], writes=[t2.b])
                P.op("dve", lambda e, o=o: e.tensor_add(out=o[:], in0=o[:], in1=t2[:]), reads=[o.b, t2.b], writes=[o.b])
                P.dma("pool", lambda e, o=o, r0=r0, g=g: e.dma_start(out=Sx["ONSA"][r0:r0 + 128, g * 256:(g + 1) * 256], in_=o[:].rearrange("p r d -> p (r d)")),
                      P.S(("o", j)), reads=[o.b], writes=[db("ONSA", ti, g)])
    P.end_phase()
    es.close()


def phase_c(C):
    nc, P, I, Sx, db = C.nc, C.P, C.I, C.Sx, C.db
    NSEQ, S, NTS = C.NSEQ, C.S, C.NTS
    es = ExitStack()
    sb, ps = mk_tiles(C, es)
    ident = sb("c_ident", [128, 128], BF16)
    identf = sb("c_identf", [128, 128])
    cw = sb("c_cw", [128, 8, 4])
    cb = sb("c_cb", [128, 8])
    dtb = sb("c_dtb", [128, 8])
    aneg = sb("c_aneg", [128, 8])
    dsk = sb("c_dsk", [128, 8])
    sng = sb("c_sng", [128, 512])
    tri = sb("c_tri", [128, 128])
    onesf = sb("c_ones", [128, 128])
    mneg = sb("c_mneg", [128, 128])
    junk = sb("c_junk", [128, 512])
    NB = 2
    xin = [sb(f"c_xin{j}", [128, 8, 131]) for j in range(NB)]
    dtt = [sb(f"c_dt{j}", [128, 8]) for j in range(NB)]
    zt = [sb(f"c_z{j}", [128, 512]) for j in range(NB)]
    acc = sb("c_acc", [128, 8, 128])
    xc = sb("c_xc", [128, 8, 128])
    bcT = sb("c_bcT", [128, 4, 128], BF16)
    bmtm = sb("c_bmtm", [128, 2, 128], BF16)
    xstm = sb("c_xstm", [128, 512])
    dte = sb("c_dte", [128, 8])
    dts = sb("c_dts", [128, 8])
    adt = sb("c_adt", [128, 8])
    acol = sb("c_acol", [128, 8])
    ea = sb("c_ea", [128, 8])
    Rr = sb("c_R", [128, 8, 128])
    alast = sb("c_alast", [128, 8])
    dsc = sb("c_dsc", [128, 8])
    cdec = sb("c_cdec", [128, 8])
    Dm = [sb(f"c_Dm{j}", [128, 128]) for j in range(2)]
    seg = [sb(f"c_seg{j}", [128, 128]) for j in range(2)]
    MT = [sb(f"c_MT{j}", [128, 128], BF16) for j in range(2)]
    xdt = sb("c_xdt", [128, 8, 64], BF16)
    xdtd = sb("c_xdtd", [128, 8, 64], BF16)
    Hst = sb("c_H", [128, 8, 64])
    Hb = sb("c_Hb", [128, 8, 64], BF16)
    yd = sb("c_yd", [128, 8, 64])
    y = sb("c_y", [128, 8, 64])
    y2 = sb("c_y2", [128, 8, 64])
    ssq = sb("c_ssq", [128, 2])
    rstd = sb("c_rstd", [128, 2])
    yo = [sb(f"c_yo{j}", [128, 512]) for j in range(NB)]
    pXT = ps("c_pXT", [128, 512])
    pBT = ps("c_pBT", [128, 512])
    pABC = [ps(f"c_pABC{j}", [128, 4, 128]) for j in range(2)]
    pCB = ps("c_pCB", [128, 2, 128])
    pYD = ps("c_pYD", [128, 8, 64])
    pYO = ps("c_pYO", [128, 8, 64])
    pST = ps("c_pST", [128, 8, 64])

    make_ident(C, ident, identf)
    for k in range(4):
        P.dma("sp", lambda e, k=k: e.dma_start(out=cw[:, :, k], in_=I["conv_w"][k, :].rearrange("(c p) -> p c", p=128), allow_slow_non_contiguous=True),
              P.S("cw"), writes=[cw.b])
    P.dma("sp", lambda e: e.dma_start(out=cb[:], in_=I["conv_b"].rearrange("(c p) o -> p (c o)", p=128), allow_slow_non_contiguous=True), P.S("cb"), writes=[cb.b])
    P.dma("sp", lambda e: e.dma_start(out=dtb[:], in_=I["dt_bias"].partition_broadcast(128)), P.S("dtb"), writes=[dtb.b])
    P.dma("sp", lambda e: e.dma_start(out=aneg[:], in_=I["a_log"].partition_broadcast(128)), P.S("aneg"), writes=[aneg.b])
    P.dma("sp", lambda e: e.dma_start(out=dsk[:], in_=I["d_skip"].partition_broadcast(128)), P.S("dsk"), writes=[dsk.b])
    P.dma("sp", lambda e: e.dma_start(out=sng[:], in_=I["ssd_norm_g"].partition_broadcast(128)), P.S("sng"), writes=[sng.b])
    P.op("act", lambda e: e.activation(out=aneg[:], in_=aneg[:], func=AF.Exp), reads=[aneg.b], writes=[aneg.b])
    P.op("dve", lambda e: e.tensor_scalar_mul(out=aneg[:], in0=aneg[:], scalar1=-1.0), reads=[aneg.b], writes=[aneg.b])
    P.op("pool", lambda e: e.memset(tri[:], 1.0), writes=[tri.b])
    P.op("pool", lambda e: e.affine_select(out=tri[:], in_=tri[:], pattern=[[1, 128]], compare_op=ALU.is_ge, fill=0.0, base=0, channel_multiplier=-1),
         reads=[tri.b], writes=[tri.b])
    P.op("pool", lambda e: e.memset(mneg[:], 0.0), writes=[mneg.b])
    P.op("pool", lambda e: e.affine_select(out=mneg[:], in_=mneg[:], pattern=[[1, 128]], compare_op=ALU.is_ge, fill=NEG, base=0, channel_multiplier=-1),
         reads=[mneg.b], writes=[mneg.b])
    P.op("pool", lambda e: e.memset(onesf[:], 1.0), writes=[onesf.b])

    cnt = 0
    for seq in range(NSEQ):
        P.op("dve", lambda e: e.memset(Hst[:], 0.0), reads=[Hst.b], writes=[Hst.b])
        P.op("dve", lambda e: e.memset(Hb[:], 0.0), reads=[Hb.b], writes=[Hb.b])
        for c in range(NTS):
            j = cnt % NB
            cnt += 1
            ti = seq * NTS + c
            r0 = ti * 128
            t0 = c * 128
            X = xin[j]
            xsrc = Sx["XBC"][seq].rearrange("(c p) t -> p c t", p=128)
            if c == 0:
                P.op("dve", lambda e, X=X: e.memset(X[:, :, 0:3], 0.0), reads=[X.b], writes=[X.b])
                P.dma("sp", lambda e, X=X, xsrc=xsrc: e.dma_start(out=X[:, :, 3:131], in_=xsrc[:, :, 0:128]), P.S(("x", j)), reads=[db("XBC", seq, 0)], writes=[X.b])
            else:
                P.dma("sp", lambda e, X=X, xsrc=xsrc, t0=t0: e.dma_start(out=X[:, :, 0:131], in_=xsrc[:, :, t0 - 3:t0 + 128]), P.S(("x", j)),
                      reads=[db("XBC", seq, c), db("XBC", seq, c - 1)], writes=[X.b])
            P.dma("sp", lambda e, j=j, r0=r0: e.dma_start(out=dtt[j][:], in_=Sx["DT"][r0:r0 + 128, :]), P.S(("dt", j)), reads=[db("DT", ti)], writes=[dtt[j].b])
            P.dma("sp", lambda e, j=j, r0=r0: e.dma_start(out=zt[j][:], in_=Sx["Z"][r0:r0 + 128, :]), P.S(("z", j)), reads=[db("Z", ti)], writes=[zt[j].b])
            for cc in range(8):
                P.op("dve", lambda e, X=X, cc=cc: e.tensor_scalar(out=acc[:, cc, :], in0=X[:, cc, 0:128], scalar1=cw[:, cc, 0:1], scalar2=cb[:, cc:cc + 1],
                                                                  op0=ALU.mult, op1=ALU.add), reads=[X.b, cw.b, cb.b, acc.b], writes=[acc.b])
                for k in range(1, 4):
                    P.op("dve", lambda e, X=X, cc=cc, k=k: e.scalar_tensor_tensor(out=acc[:, cc, :], in0=X[:, cc, k:k + 128], scalar=cw[:, cc, k:k + 1], in1=acc[:, cc, :],
                                                                                  op0=ALU.mult, op1=ALU.add), reads=[X.b, cw.b, acc.b], writes=[acc.b])
            P.op("act", lambda e: e.activation(out=xc[:], in_=acc[:], func=AF.Silu), reads=[acc.b], writes=[xc.b])
            P.op("dve", lambda e: e.tensor_copy(out=bcT[:], in_=xc[:, 4:8, :]), reads=[xc.b], writes=[bcT.b])
            for cc in range(4):
                P.op("pe", lambda e, cc=cc: e.transpose(out=pXT[:, cc * 128:(cc + 1) * 128], in_=xc[:, cc, :], identity=identf[:]), reads=[xc.b, identf.b], writes=[pXT.b])
            P.op("act", lambda e: e.activation(out=xstm[:], in_=pXT[:], func=AF.Copy), reads=[pXT.b], writes=[xstm.b])
            for g in range(2):
                P.op("pe", lambda e, g=g: e.transpose(out=pBT[:, g * 128:(g + 1) * 128], in_=xc[:, 4 + g, :], identity=identf[:]), reads=[xc.b, identf.b], writes=[pBT.b])
            P.op("act", lambda e: e.activation(out=bmtm[:], in_=pBT[:, 0:256].rearrange("p (g n) -> p g n", g=2), func=AF.Copy), reads=[pBT.b], writes=[bmtm.b])
            P.op("dve", lambda e, j=j: e.tensor_add(out=dte[:], in0=dtt[j][:], in1=dtb[:]), reads=[dtt[j].b, dtb.b], writes=[dte.b])
            P.op("act", lambda e: e.activation(out=dte[:], in_=dte[:], func=AF.Exp), reads=[dte.b], writes=[dte.b])
            P.op("act", lambda e: e.activation(out=dts[:], in_=dte[:], func=AF.Ln, bias=1.0), reads=[dte.b], writes=[dts.b])
            P.op("dve", lambda e: e.tensor_mul(out=adt[:], in0=dts[:], in1=aneg[:]), reads=[dts.b, aneg.b], writes=[adt.b])
            P.op("pe", lambda e: e.matmul(pBT[:, 256:264], lhsT=tri[:], rhs=adt[:], start=True, stop=True), reads=[tri.b, adt.b], writes=[pBT.b])
            P.op("dve", lambda e: e.tensor_copy(out=acol[:], in_=pBT[:, 256:264]), reads=[pBT.b], writes=[acol.b])
            P.op("act", lambda e: e.activation(out=ea[:], in_=pBT[:, 256:264], func=AF.Exp), reads=[pBT.b], writes=[ea.b])
            for h in range(8):
                P.op("dve", lambda e, h=h: e.tensor_scalar_mul(out=Rr[:, h, :], in0=tri[:], scalar1=adt[:, h:h + 1]), reads=[tri.b, adt.b, Rr.b], writes=[Rr.b])
            for hh in range(2):
                P.op("pe", lambda e, hh=hh: e.matmul(pABC[hh][:].rearrange("p h l -> p (h l)"), lhsT=onesf[:], rhs=Rr[:, hh * 4:(hh + 1) * 4, :].rearrange("p h l -> p (h l)"),
                                                    start=True, stop=True), reads=[onesf.b, Rr.b], writes=[pABC[hh].b])
            for hh in range(2):
                P.op("dve", lambda e, hh=hh: e.tensor_copy(out=alast[:, hh * 4:(hh + 1) * 4], in_=pABC[hh][:, :, 127]), reads=[pABC[hh].b, alast.b], writes=[alast.b])
            P.op("dve", lambda e: e.tensor_sub(out=dsc[:], in0=alast[:], in1=acol[:]), reads=[alast.b, acol.b], writes=[dsc.b])
            P.op("act", lambda e: e.activation(out=dsc[:], in_=dsc[:], func=AF.Exp), reads=[dsc.b], writes=[dsc.b])
            P.op("act", lambda e: e.activation(out=cdec[:], in_=alast[:], func=AF.Exp), reads=[alast.b], writes=[cdec.b])
            xs3 = xstm[:].rearrange("p (h d) -> p h d", d=64)
            P.op("dve", lambda e, xs3=xs3: e.tensor_mul(out=xdt[:], in0=xs3, in1=dts[:].unsqueeze(2).to_broadcast([128, 8, 64])), reads=[xstm.b, dts.b], writes=[xdt.b])
            P.op("dve", lambda e: e.tensor_mul(out=xdtd[:], in0=xdt[:], in1=dsc[:].unsqueeze(2).to_broadcast([128, 8, 64])), reads=[xdt.b, dsc.b], writes=[xdtd.b])
            for g in range(2):
                P.op("pe", lambda e, g=g: e.matmul(pCB[:, g, :], lhsT=bcT[:, g, :], rhs=bcT[:, 2 + g, :], start=True, stop=True), reads=[bcT.b], writes=[pCB.b])
            for h in range(8):
                g = h // 4
                k2 = h % 2
                P.op("dve", lambda e, h=h, k2=k2: e.scalar_tensor_tensor(out=Dm[k2][:], in0=pABC[h // 4][:, h % 4, :], scalar=acol[:, h:h + 1], in1=mneg[:],
                                                                         op0=ALU.subtract, op1=ALU.add), reads=[pABC[h // 4].b, acol.b, mneg.b], writes=[Dm[k2].b])
                P.op("act", lambda e, k2=k2: e.activation(out=seg[k2][:], in_=Dm[k2][:], func=AF.Exp), reads=[Dm[k2].b], writes=[seg[k2].b])
                P.op("dve", lambda e, g=g, k2=k2: e.tensor_mul(out=MT[k2][:], in0=pCB[:, g, :], in1=seg[k2][:]), reads=[pCB.b, seg[k2].b], writes=[MT[k2].b])
                P.op("pe", lambda e, h=h, k2=k2: e.matmul(pYD[:, h, :], lhsT=MT[k2][:], rhs=xdt[:, h, :], start=True, stop=True), reads=[MT[k2].b, xdt.b], writes=[pYD.b])
                P.op("pe", lambda e, h=h, g=g: e.matmul(pYO[:, h, :], lhsT=bcT[:, 2 + g, :], rhs=Hb[:, h, :], start=True, stop=True), reads=[bcT.b, Hb.b], writes=[pYO.b])
                P.op("pe", lambda e, h=h, g=g: e.matmul(pST[:, h, :], lhsT=bmtm[:, g, :], rhs=xdtd[:, h, :], start=True, stop=True), reads=[bmtm.b, xdtd.b], writes=[pST.b])
            P.op("act", lambda e: e.activation(out=yd[:], in_=pYD[:], func=AF.Copy), reads=[pYD.b], writes=[yd.b])
            P.op("dve", lambda e: e.tensor_mul(out=y[:], in0=pYO[:], in1=ea[:].unsqueeze(2).to_broadcast([128, 8, 64])), reads=[pYO.b, ea.b], writes=[y.b])
            P.op("dve", lambda e: e.tensor_add(out=y[:], in0=y[:], in1=yd[:]), reads=[y.b, yd.b], writes=[y.b])
            P.op("dve", lambda e, xs3=xs3: e.tensor_mul(out=y2[:], in0=xs3, in1=dsk[:].unsqueeze(2).to_broadcast([128, 8, 64])), reads=[xstm.b, dsk.b], writes=[y2.b])
            P.op("dve", lambda e: e.tensor_add(out=y[:], in0=y[:], in1=y2[:]), reads=[y.b, y2.b], writes=[y.b])
            yf = y[:].rearrange("p h d -> p (h d)")
            P.op("dve", lambda e, yf=yf, j=j: e.tensor_mul(out=yf, in0=yf, in1=zt[j][:]), reads=[y.b, zt[j].b], writes=[y.b])
            for g in range(2):
                P.op("act", lambda e, g=g, yf=yf: e.activation(out=junk[:, 0:256], in_=yf[:, g * 256:(g + 1) * 256], func=AF.Square, accum_out=ssq[:, g:g + 1]),
                     reads=[y.b, ssq.b], writes=[ssq.b])
            P.op("act", lambda e: e.activation(out=rstd[:], in_=ssq[:], func=AF.Sqrt, scale=1.0 / 256, bias=EPS), reads=[ssq.b], writes=[rstd.b])
            P.op("dve", lambda e: e.reciprocal(out=rstd[:], in_=rstd[:]), reads=[rstd.b], writes=[rstd.b])
            for g in range(2):
                P.op("dve", lambda e, g=g, j=j, yf=yf: e.scalar_tensor_tensor(out=yo[j][:, g * 256:(g + 1) * 256], in0=yf[:, g * 256:(g + 1) * 256], scalar=rstd[:, g:g + 1],
                                                                             in1=sng[:, g * 256:(g + 1) * 256], op0=ALU.mult, op1=ALU.mult),
                     reads=[y.b, rstd.b, sng.b, yo[j].b], writes=[yo[j].b])
            P.dma("pool", lambda e, j=j, r0=r0: e.dma_start(out=Sx["MIX"][r0:r0 + 128, 512:1024], in_=yo[j][:]), P.S(("o", j)), reads=[yo[j].b], writes=[db("MIXS", ti)])
            P.op("dve", lambda e: e.tensor_mul(out=Hst[:], in0=Hst[:], in1=cdec[:].unsqueeze(2).to_broadcast([128, 8, 64])), reads=[Hst.b, cdec.b], writes=[Hst.b])
            P.op("dve", lambda e: e.tensor_add(out=Hst[:], in0=Hst[:], in1=pST[:]), reads=[Hst.b, pST.b], writes=[Hst.b])
            P.op("act", lambda e: e.activation(out=Hb[:], in_=Hst[:], func=AF.Copy), reads=[Hst.b], writes=[Hb.b])
    P.end_phase()
    es.close()


def phase_d(C):
    nc, P, I, Sx, db = C.nc, C.P, C.I, C.Sx, C.db
    NSEQ, S, NTS, T = C.NSEQ, C.S, C.NTS, C.T
    es = ExitStack()
    sb, ps = mk_tiles(C, es)
    ident = sb("d_ident", [128, 128], BF16)
    identf = sb("d_identf", [128, 128])
    Wout = sb("d_Wout", [128, 8, 1024], BF16)
    Wq = sb("d_Wq", [128, 8, 2048], BF16)
    keysT = sb("d_keysT", [128, 16, 128], BF16)
    nsag = sb("d_nsag", [128, 512])
    ffng = sb("d_ffng", [128, 1024])
    fing = sb("d_fing", [128, 1024])
    onsa = sb("d_onsa", [128, 512])
    mixs = sb("d_mixs", [128, 512])
    mixb = sb("d_mixb", [128, 1024], BF16)
    mixT = sb("d_mixT", [128, 8, 128], BF16)
    xt = sb("d_xt", [128, 1024])
    acc = sb("d_acc", [128, 1024])
    hn = sb("d_hn", [128, 1024])
    hnB = sb("d_hnB", [128, 1024])
    hnbs = [sb(f"d_hnb{j}", [128, 1024], BF16) for j in range(2)]
    prodb = [sb(f"d_prodb{j}", [128, 1024], BF16) for j in range(2)]
    hnT = mixT
    qT = sb("d_qT", [128, 16, 128], BF16)
    sc = sb("d_sc", [128, 16, 128])
    sc2 = sb("d_sc2", [128, 16, 128])
    m16 = sb("d_m16", [128, 16, 16])
    i16u = sb("d_i16u", [128, 16, 16], U32)
    i16f = sb("d_i16f", [128, 16, 16])
    cand = View(sc[:].rearrange("p (h x) k -> p h (x k)", h=8), sc.b)
    abu = sb("d_abu", [128, 2, 128], U32)
    abf = sb("d_abf", [128, 2, 128])
    e12 = sb("d_e12", [128, 2, 128])
    ug = sb("d_ug", [128, 128])
    vals = sb("d_vals", [128, 8, 16])
    posu = sb("d_posu", [128, 8, 16], U32)
    iota_i = sb("d_iota_i", [128, 256], I32)
    iota_f = sb("d_iota_f", [128, 256])
    eidf = sb("d_eidf", [128, 128])
    eidi = sb("d_eidi", [128, 128], I32)
    junk = sb("d_junk", [128, 1024])
    u = sb("d_u", [128, 128])
    g1 = sb("d_g1", [128, 128])
    g2 = sb("d_g2", [128, 128])
    gate = sb("d_gate", [128, 8, 16])
    gsum = sb("d_gsum", [128, 8])
    gh = sb("d_gh", [128, 128])
    epsb = sb("d_epsb", [128, 1])
    ssq = sb("d_ssq", [128, 1])
    rstd = sb("d_rstd", [128, 1])
    NG = 16
    GS = 4
    Guv = [sb(f"d_Guv{j}", [128, 2048], BF16) for j in range(NG)]

    ot = xt
    pT = ps("d_pT", [128, 8, 128], BF16)
    pO = [ps(f"d_pO{j}", [128, 512]) for j in range(2)]
    pQ = [ps(f"d_pQ{j}", [128, 4, 128]) for j in range(2)]
    pV = [ps(f"d_pV{j}", [128, 512]) for j in range(2)]
    diags = [sb(f"d_diag{j}", [128, 128], BF16) for j in range(8)]

    make_ident(C, ident, identf)
    UV, tab = C.UV, C.tab
    P.op("dve", lambda e: e.memset(epsb[:], EPS), writes=[epsb.b])
    P.op("pool", lambda e: e.iota(iota_i[:], pattern=[[1, 256]], base=0, channel_multiplier=0), writes=[iota_i.b])
    P.op("dve", lambda e: e.tensor_copy(out=iota_f[:], in_=iota_i[:]), reads=[iota_i.b], writes=[iota_f.b])
    wo = I["w_out"].rearrange("(c p) n -> p c n", p=128)
    wq = I["peer_w_q"].rearrange("(c p) n -> p c n", p=128)
    for c in range(8):
        P.dma("pool", lambda e, c=c: e.dma_start(out=Wout[:, c, :], in_=wo[:, c, :]), P.S("wout"), writes=[Wout.b])
        P.dma("pool", lambda e, c=c: e.dma_start(out=Wq[:, c, :], in_=wq[:, c, :]), P.S("wq"), writes=[Wq.b])
    P.dma("sp", lambda e: e.dma_start(out=nsag[:], in_=I["nsa_norm_g"].partition_broadcast(128)), P.S("nsag"), writes=[nsag.b])
    P.dma("sp", lambda e: e.dma_start(out=ffng[:], in_=I["ffn_norm_g"].partition_broadcast(128)), P.S("ffng"), writes=[ffng.b])
    P.dma("sp", lambda e: e.dma_start(out=fing[:], in_=I["final_norm_g"].partition_broadcast(128)), P.S("fing"), writes=[fing.b])
    P.dma("sp", lambda e: e.dma_start(out=sc[:], in_=I["peer_keys"].rearrange("a k d -> k a d")), P.S("keys"), writes=[sc.b])
    for b4 in range(4):
        for a in range(b4 * 4, b4 * 4 + 4):
            P.op("pe", lambda e, a=a, b4=b4: e.transpose(out=pQ[b4 % 2][:, a % 4, :], in_=sc[:, a, :], identity=identf[:]), reads=[sc.b, identf.b], writes=[pQ[b4 % 2].b])
        P.op("act", lambda e, b4=b4: e.activation(out=keysT[:, b4 * 4:(b4 + 1) * 4, :], in_=pQ[b4 % 2][:], func=AF.Copy), reads=[pQ[b4 % 2].b, keysT.b], writes=[keysT.b])

    def rms(src_ap, srcb, n):
        P.op("act", lambda e: e.activation(out=junk[:, 0:n], in_=src_ap, func=AF.Square, accum_out=ssq[:]), reads=[srcb], writes=[ssq.b])
        P.op("act", lambda e: e.activation(out=rstd[:], in_=ssq[:], func=AF.Ln, scale=1.0 / n, bias=epsb[:]), reads=[ssq.b, epsb.b], writes=[rstd.b])
        P.op("act", lambda e: e.activation(out=rstd[:], in_=rstd[:], func=AF.Exp, scale=-0.5), reads=[rstd.b], writes=[rstd.b])

    accs = [acc, sb('d_accB', [128, 1024])]
    eidis = [eidi, sb('d_eidiB', [128, 128], I32)]

    hns = [hn, hnB]

    def stage1(ti):
        acc, eidi, hn, hnb = accs[ti % 2], eidis[ti % 2], hns[ti % 2], hnbs[ti % 2]
        r0 = ti * 128
        P.dma("sp", lambda e, r0=r0: e.dma_start(out=onsa[:], in_=Sx["ONSA"][r0:r0 + 128, :]), P.S("onsa"), reads=[db("ONSA", ti, 0), db("ONSA", ti, 1)], writes=[onsa.b])
        P.dma("sp", lambda e, r0=r0: e.dma_start(out=mixs[:], in_=Sx["MIX"][r0:r0 + 128, 512:1024]), P.S("mixs"), reads=[db("MIXS", ti)], writes=[mixs.b])
        P.dma("sp", lambda e, r0=r0: e.dma_start(out=xt[:], in_=I["x"][r0:r0 + 128, :]), P.S("xt"), writes=[xt.b])
        rms(onsa[:], onsa.b, 512)
        P.op("dve", lambda e: e.scalar_tensor_tensor(out=mixb[:, 0:512], in0=onsa[:], scalar=rstd[:, 0:1], in1=nsag[:], op0=ALU.mult, op1=ALU.mult),
             reads=[onsa.b, rstd.b, nsag.b, mixb.b], writes=[mixb.b])
        P.op("act", lambda e: e.activation(out=mixb[:, 512:1024], in_=mixs[:], func=AF.Copy), reads=[mixs.b, mixb.b], writes=[mixb.b])
        for c in range(8):
            P.op("pe", lambda e, c=c: e.transpose(out=pT[:, c, :], in_=mixb[:, c * 128:(c + 1) * 128], identity=ident[:]), reads=[mixb.b, ident.b], writes=[pT.b])
        P.op("act", lambda e: e.activation(out=mixT[:], in_=pT[:], func=AF.Copy), reads=[pT.b], writes=[mixT.b])
        for n2 in range(2):
            for c in range(8):
                P.op("pe", lambda e, n2=n2, c=c: e.matmul(pO[n2][:], lhsT=mixT[:, c, :], rhs=Wout[:, c, n2 * 512:(n2 + 1) * 512], start=(c == 0), stop=(c == 7)),
                     reads=[mixT.b, Wout.b], writes=[pO[n2].b])
        for n2 in range(2):
            P.op("dve", lambda e, n2=n2: e.tensor_add(out=acc[:, n2 * 512:(n2 + 1) * 512], in0=pO[n2][:], in1=xt[:, n2 * 512:(n2 + 1) * 512]),
                 reads=[pO[n2].b, xt.b, acc.b], writes=[acc.b])
        rms(acc[:], acc.b, 1024)
        P.op("dve", lambda e: e.scalar_tensor_tensor(out=hn[:], in0=acc[:], scalar=rstd[:, 0:1], in1=ffng[:], op0=ALU.mult, op1=ALU.mult),
             reads=[acc.b, rstd.b, ffng.b], writes=[hn.b])
        P.op("act", lambda e: e.activation(out=hnb[:], in_=hn[:], func=AF.Copy), reads=[hn.b], writes=[hnb.b])
        for c in range(8):
            P.op("pe", lambda e, c=c: e.transpose(out=pT[:, c, :], in_=hnb[:, c * 128:(c + 1) * 128], identity=ident[:]), reads=[hnb.b, ident.b], writes=[pT.b])
        P.op("act", lambda e: e.activation(out=hnT[:], in_=pT[:], func=AF.Copy), reads=[pT.b], writes=[hnT.b])
        for b4 in range(4):
            pq = pQ[b4 % 2]
            for hc in range(b4 * 4, b4 * 4 + 4):
                for c in range(8):
                    P.op("pe", lambda e, hc=hc, c=c, pq=pq: e.matmul(pq[:, hc % 4, :], lhsT=Wq[:, c, hc * 128:(hc + 1) * 128], rhs=hnT[:, c, :], start=(c == 0), stop=(c == 7)),
                         reads=[Wq.b, hnT.b], writes=[pq.b])
            P.op("act", lambda e, b4=b4, pq=pq: e.activation(out=qT[:, b4 * 4:(b4 + 1) * 4, :], in_=pq[:], func=AF.Copy), reads=[pq.b, qT.b], writes=[qT.b])
        for grp in range(4):
            pb = pO[grp % 2]
            for k4 in range(4):
                hc = grp * 4 + k4
                P.op("pe", lambda e, pb=pb, k4=k4, hc=hc: e.matmul(pb[:, k4 * 128:(k4 + 1) * 128], lhsT=qT[:, hc, :], rhs=keysT[:, hc, :], start=True, stop=True),
                     reads=[qT.b, keysT.b], writes=[pb.b])
            P.op("act", lambda e, pb=pb, grp=grp: e.activation(out=sc[:, grp * 4:(grp + 1) * 4, :], in_=pb[:].rearrange("p (a k) -> p a k", a=4), func=AF.Copy),
                 reads=[pb.b, sc.b], writes=[sc.b])
        for hc in range(16):
            P.op("dve", lambda e, hc=hc: e.max(out=m16[:, hc, 0:8], in_=sc[:, hc, :]), reads=[sc.b, m16.b], writes=[m16.b])
            P.op("dve", lambda e, hc=hc: e.max_index(out=i16u[:, hc, 0:8], in_max=m16[:, hc, 0:8], in_values=sc[:, hc, :]), reads=[sc.b, m16.b, i16u.b], writes=[i16u.b])
            P.op("dve", lambda e, hc=hc: e.match_replace(out=sc2[:, hc, :], in_to_replace=m16[:, hc, 0:8], in_values=sc[:, hc, :], imm_value=-1e30),
                 reads=[sc.b, m16.b, sc2.b], writes=[sc2.b])
            P.op("dve", lambda e, hc=hc: e.max(out=m16[:, hc, 8:16], in_=sc2[:, hc, :]), reads=[sc2.b, m16.b], writes=[m16.b])
            P.op("dve", lambda e, hc=hc: e.max_index(out=i16u[:, hc, 8:16], in_max=m16[:, hc, 8:16], in_values=sc2[:, hc, :]), reads=[sc2.b, m16.b, i16u.b], writes=[i16u.b])
        P.op("dve", lambda e: e.tensor_copy(out=i16f[:], in_=i16u[:]), reads=[i16u.b], writes=[i16f.b])
        c4 = cand[:].rearrange("p h (a b) -> p h a b", a=16)
        P.op("dve", lambda e, c4=c4: e.tensor_tensor(out=c4, in0=m16[:, 0::2, :].unsqueeze(3).to_broadcast([128, 8, 16, 16]),
                                                     in1=m16[:, 1::2, :].unsqueeze(2).to_broadcast([128, 8, 16, 16]), op=ALU.add), reads=[m16.b], writes=[cand.b])
        cand2 = sc2[:].rearrange("p (h x) k -> p h (x k)", h=8)
        for h in range(8):
            P.op("dve", lambda e, h=h: e.max(out=vals[:, h, 0:8], in_=cand[:, h, :]), reads=[cand.b, vals.b], writes=[vals.b])
            P.op("dve", lambda e, h=h: e.max_index(out=posu[:, h, 0:8], in_max=vals[:, h, 0:8], in_values=cand[:, h, :]), reads=[cand.b, vals.b, posu.b], writes=[posu.b])
            P.op("dve", lambda e, h=h, cand2=cand2: e.match_replace(out=cand2[:, h, :], in_to_replace=vals[:, h, 0:8], in_values=cand[:, h, :], imm_value=-1e30),
                 reads=[cand.b, vals.b, sc2.b], writes=[sc2.b])
            P.op("dve", lambda e, h=h, cand2=cand2: e.max(out=vals[:, h, 8:16], in_=cand2[:, h, :]), reads=[sc2.b, vals.b], writes=[vals.b])
            P.op("dve", lambda e, h=h, cand2=cand2: e.max_index(out=posu[:, h, 8:16], in_max=vals[:, h, 8:16], in_values=cand2[:, h, :]), reads=[sc2.b, vals.b, posu.b], writes=[posu.b])
        pflat = posu[:].rearrange("p h k -> p (h k)")
        P.op("dve", lambda e, pflat=pflat: e.tensor_single_scalar(out=abu[:, 0, :], in_=pflat, scalar=4, op=ALU.logical_shift_right), reads=[posu.b, abu.b], writes=[abu.b])
        P.op("dve", lambda e, pflat=pflat: e.tensor_single_scalar(out=abu[:, 1, :], in_=pflat, scalar=15, op=ALU.bitwise_and), reads=[posu.b, abu.b], writes=[abu.b])
        P.op("dve", lambda e: e.tensor_copy(out=abf[:], in_=abu[:]), reads=[abu.b], writes=[abf.b])
        T4 = sc2[:].rearrange("p a k -> p (a k)").rearrange("p (h k a) -> p h k a", h=8, k=16)
        io16 = iota_f[:, 0:16].unsqueeze(1).unsqueeze(1).to_broadcast([128, 8, 16, 16])
        for w in range(2):
            sel = abf[:, w, :].rearrange("p (h k) -> p h k", h=8).unsqueeze(3).to_broadcast([128, 8, 16, 16])
            tabv = i16f[:, w::2, :].unsqueeze(2).to_broadcast([128, 8, 16, 16])
            P.op("dve", lambda e, sel=sel, T4=T4, io16=io16: e.tensor_tensor(out=T4, in0=sel, in1=io16, op=ALU.is_equal), reads=[abf.b, iota_f.b, sc2.b], writes=[sc2.b])
            P.op("dve", lambda e, T4=T4, tabv=tabv: e.tensor_tensor(out=T4, in0=T4, in1=tabv, op=ALU.mult), reads=[sc2.b, i16f.b], writes=[sc2.b])
            P.op("dve", lambda e, T4=T4, w=w: e.tensor_reduce(out=e12[:, w, :].rearrange("p (h k) -> p h k", h=8), in_=T4, axis=AX.X, op=ALU.add),
                 reads=[sc2.b, e12.b], writes=[e12.b])
        P.op("dve", lambda e: e.scalar_tensor_tensor(out=eidf[:], in0=e12[:, 0, :], scalar=128.0, in1=e12[:, 1, :], op0=ALU.mult, op1=ALU.add),
             reads=[e12.b], writes=[eidf.b])
        P.op("dve", lambda e: e.tensor_copy(out=eidi[:], in_=eidf[:]), reads=[eidf.b], writes=[eidi.b])


    junkp = sb("d_junkp", [128, 1024])
    ub = [Buf(f"u{g}") for g in range(128 // GS)]
    ub2 = [Buf(f"u2{g}") for g in range(128 // GS)]
    g1b = [Buf(f"g1{g}") for g in range(128 // GS)]
    ghb = [Buf(f"gh{g}") for g in range(128 // GS)]

    def gphase(ti, thunks):
        acc, eidi, hn, hnb = accs[ti % 2], eidis[ti % 2], hns[ti % 2], hnbs[ti % 2]
        r0 = ti * 128
        NGRP = 128 // GS
        per = (len(thunks) + 127) // 128
        P.op("dve", lambda e: e.tensor_sub(out=gate[:], in0=vals[:], in1=vals[:, :, 0:1].to_broadcast([128, 8, 16])), reads=[vals.b], writes=[gate.b])
        P.op("act", lambda e: e.activation(out=gate[:], in_=gate[:], func=AF.Exp), reads=[gate.b], writes=[gate.b])
        P.op("dve", lambda e: e.tensor_reduce(out=gsum[:], in_=gate[:], axis=AX.X, op=ALU.add), reads=[gate.b], writes=[gsum.b])
        P.op("dve", lambda e: e.reciprocal(out=gsum[:], in_=gsum[:]), reads=[gsum.b], writes=[gsum.b])
        P.op("dve", lambda e: e.tensor_mul(out=gate[:], in0=gate[:], in1=gsum[:].unsqueeze(2).to_broadcast([128, 8, 16])), reads=[gate.b, gsum.b], writes=[gate.b])
        gflat = gate[:].rearrange("p h k -> p (h k)")
        for step in range(NGRP + 2):
            if step >= 2:
                g = step - 2
                cs = slice(g * GS, (g + 1) * GS)
                P.op("dve", lambda e, cs=cs: e.tensor_mul(out=gh[:, cs], in0=g2[:, cs], in1=ug[:, cs]), reads=[g1b[g], ghb[g]], writes=[ghb[g]])
                for s_ in range(GS):
                    jx = g * GS + s_
                    G = Guv[jx % NG]
                    dg = diags[jx % 8]
                    P.op("act", lambda e, jx=jx, dg=dg: e.activation(out=dg[:], in_=ident[:], func=AF.Copy, scale=gh[:, jx:jx + 1]), reads=[ident.b, ghb[g]], writes=[dg.b])
                    for n2 in range(2):
                        P.op("pe", lambda e, dg=dg, G=G, n2=n2, jx=jx: e.matmul(pV[n2][:], lhsT=dg[:], rhs=G[:, 1024 + n2 * 512:1024 + (n2 + 1) * 512], start=(jx == 0), stop=(jx == 127)),
                             reads=[dg.b, G.b], writes=[pV[n2].b])
            if step < NGRP:
                for s_ in range(GS):
                    jx = step * GS + s_
                    G = Guv[jx % NG]
                    P.dma("pool", lambda e, G=G, jx=jx, eidi=eidi: e.indirect_dma_start(out=G[:], out_offset=None, in_=UV[:, :],
                                                                                      in_offset=bass.IndirectOffsetOnAxis(ap=eidi[:, jx:jx + 1], axis=0)),
                          P.S(("g", jx % NG)), reads=[eidi.b, tab], writes=[G.b])
                for s_ in range(GS):
                    jx = step * GS + s_
                    G = Guv[jx % NG]
                    if s_ == GS - 1 and POOL_SHARE:
                        P.op("pool", lambda e, G=G, hn=hn: e.tensor_tensor(out=junkp[:], in0=G[:, 0:1024], in1=hn[:], op=ALU.mult), reads=[G.b, hn.b], writes=[junkp.b])
                        P.op("act", lambda e, jx=jx: e.activation(out=junkp[:], in_=junkp[:], func=AF.Copy, accum_out=u[:, jx:jx + 1]), reads=[junkp.b], writes=[junkp.b], nodep=[ub[step]])
                        continue
                    if False:
                        P.op("dve", lambda e, G=G, jx=jx, hn=hn: e.scalar_tensor_tensor(out=junk[:], in0=G[:, 0:1024], scalar=1.0, in1=hn[:], op0=ALU.mult, op1=ALU.mult,
                                                                                       accum_out=u[:, jx:jx + 1]),
                             reads=[G.b, hn.b], nodep=[ub2[step]])
                        if thunks:
                            P.replay(thunks[:per])
                            del thunks[:per]
                        continue
                    pb_ = prodb[jx % 2]
                    P.op("dve", lambda e, G=G, hnb=hnb, pb_=pb_: e.tensor_tensor(out=pb_[:], in0=G[:, 0:1024], in1=hnb[:], op=ALU.mult), reads=[G.b, hnb.b], writes=[pb_.b])
                    P.op("act", lambda e, pb_=pb_, jx=jx: e.activation(out=pb_[:], in_=pb_[:], func=AF.Copy, accum_out=u[:, jx:jx + 1]),
                         reads=[pb_.b], writes=([pb_.b, ub[step]] if s_ == 0 else [pb_.b]), nodep=([] if s_ == 0 else [ub[step]]))
                    if thunks:
                        P.replay(thunks[:per])
                        del thunks[:per]
            if 1 <= step <= NGRP:
                g = step - 1
                cs = slice(g * GS, (g + 1) * GS)
                P.op("dve", lambda e, cs=cs: e.tensor_mul(out=g1[:, cs], in0=u[:, cs], in1=u[:, cs]), reads=[ub[g], ub2[g]], writes=[g1b[g]])
                P.op("dve", lambda e, cs=cs: e.tensor_scalar(out=g1[:, cs], in0=g1[:, cs], scalar1=0.044715, scalar2=1.0, op0=ALU.mult, op1=ALU.add), reads=[g1b[g]], writes=[g1b[g]])
                P.op("dve", lambda e, cs=cs: e.tensor_mul(out=g1[:, cs], in0=g1[:, cs], in1=u[:, cs]), reads=[g1b[g], ub[g]], writes=[g1b[g]])
                P.op("dve", lambda e, cs=cs, gflat=gflat: e.tensor_mul(out=ug[:, cs], in0=u[:, cs], in1=gflat[:, cs]), reads=[ub[g], gate.b, ghb[g]], writes=[ghb[g]])
                P.op("act", lambda e, cs=cs: e.activation(out=g2[:, cs], in_=g1[:, cs], func=AF.Sigmoid, scale=1.5957691216057308), reads=[g1b[g]], writes=[g1b[g]])
        if thunks:
            P.replay(thunks)
        for n2 in range(2):
            P.op("dve", lambda e, n2=n2, acc=acc: e.tensor_add(out=acc[:, n2 * 512:(n2 + 1) * 512], in0=pV[n2][:], in1=acc[:, n2 * 512:(n2 + 1) * 512]),
                 reads=[pV[n2].b, acc.b], writes=[acc.b])
        rms(acc[:], acc.b, 1024)
        P.op("dve", lambda e, acc=acc: e.scalar_tensor_tensor(out=ot[:], in0=acc[:], scalar=rstd[:, 0:1], in1=fing[:], op0=ALU.mult, op1=ALU.mult),
             reads=[acc.b, rstd.b, fing.b], writes=[ot.b])
        P.dma("sp", lambda e, r0=r0: e.dma_start(out=C.out[r0:r0 + 128, :], in_=ot[:]), P.S("out"), reads=[ot.b], writes=[db("OUT", ti)])

    def record_stage1(ti):
        P.defer = []
        stage1(ti)
        th = P.defer
        P.defer = None
        return th

    NT = T // 128
    stage1(0)
    for ti in range(NT):
        th = record_stage1(ti + 1) if ti + 1 < NT else []
        gphase(ti, th)

    P.end_phase()
    es.close()
```

```python
from contextlib import ExitStack
import numpy as np
import ml_dtypes
import concourse.bass as bass
import concourse.mybir as mybir
from concourse.bass_utils import run_bass_kernel_spmd

F32 = mybir.dt.float32
BF16 = mybir.dt.bfloat16
I32 = mybir.dt.int32
U32 = mybir.dt.uint32
ALU = mybir.AluOpType
AF = mybir.ActivationFunctionType
AX = mybir.AxisListType

D = 1024
NH = 8
HD = 64
INP = 2848
EPS = 1e-6
NEG = -30000.0
POOL_SHARE = False


class Sem:
    def __init__(self, h, name):
        self.h = h
        self.cnt = 0
        self.name = name


class Buf:
    __slots__ = ("name", "w", "r")

    def __init__(self, name=""):
        self.name = name
        self.w = None
        self.r = {}


class Prog:
    EPOCH = 30000

    def __init__(self, nc, es):
        self.nc = nc
        self.es = es
        self.engs = ["pe", "act", "dve", "pool", "sp"]
        self.streams = {e: [] for e in self.engs}
        self.seq = {e: 0 for e in self.engs}
        self.esems = {e: [] for e in self.engs}
        self.known = {e: {} for e in self.engs}
        self.nsem = 0
        self.dsems = []
        self.phase_sems = {}
        self.free_sems = {}
        self.defer = None

    def newsem(self, name):
        self.nsem += 1
        h = self.es.enter_context(self.nc.semaphore(f"{name}_{self.nsem}"))
        return Sem(h, name)

    def dsem(self, name="d"):
        s = self.newsem(name)
        self.dsems.append(s)
        return s

    def S(self, key):
        return ("SEMKEY", key)

    def _resolve_sem(self, key, q):
        cls = "sw" if q == "pool" else "hw"
        k = (cls, key)
        if k not in self.phase_sems:
            fl = self.free_sems.setdefault(cls, [])
            sm = fl.pop() if fl else self.dsem("d" + cls)
            self.phase_sems[k] = sm
        return self.phase_sems[k]

    def end_phase(self):
        self.barrier()
        for (cls, _), sm in self.phase_sems.items():
            self.free_sems.setdefault(cls, []).append(sm)
        self.phase_sems = {}

    def _etok(self, e, k):
        idx = k // self.EPOCH
        while len(self.esems[e]) <= idx:
            self.esems[e].append(self.newsem(f"e{e}"))
        return (self.esems[e][idx], k % self.EPOCH + 1, e)

    def _waits(self, e, deps, skip_sem=None):
        need = {}
        for tok in deps:
            if tok is None:
                continue
            sem, val, src = tok
            if skip_sem is not None and sem is skip_sem:
                continue
            if e == "pe" and src == "pe":
                continue
            if self.known[e].get(sem, 0) >= val:
                continue
            if need.get(sem, 0) < val:
                need[sem] = val
        for sem, val in need.items():
            self.known[e][sem] = val
            self.streams[e].append(("wait", sem, val))

    def _deps(self, reads, writes):
        deps = []
        for b in reads:
            deps.append(b.w)
        for b in writes:
            deps.append(b.w)
            for s, (v, src) in b.r.items():
                deps.append((s, v, src))
        return deps

    def _mark(self, tok, reads, writes):
        sem, val, src = tok
        for b in reads:
            b.r[sem] = (val, src)
        for b in writes:
            b.w = tok
            b.r = {}

    def op(self, e, fn, reads=(), writes=(), nodep=()):
        if self.defer is not None:
            self.defer.append(("op", e, fn, tuple(reads), tuple(writes), tuple(nodep)))
            return None
        self._waits(e, self._deps(reads, writes))
        k = self.seq[e]
        self.seq[e] += 1
        tok = self._etok(e, k)
        self.streams[e].append(("op", fn, tok[0], 1))
        self._mark(tok, reads, writes)
        for b in nodep:
            b.w = tok
        return tok

    def dma(self, q, fn, sem, reads=(), writes=()):
        if self.defer is not None:
            self.defer.append(("dma", q, fn, sem, tuple(reads), tuple(writes)))
            return None
        if isinstance(sem, tuple):
            sem = self._resolve_sem(sem[1], q)
        self._waits(q, self._deps(reads, writes), skip_sem=sem)
        sem.cnt += 16
        tok = (sem, sem.cnt, "dma")
        self.streams[q].append(("op", fn, sem, 16))
        self._mark(tok, reads, writes)
        return tok

    def replay(self, items):
        for it in items:
            if it[0] == "op":
                self.op(it[1], it[2], it[3], it[4], it[5])
            else:
                self.dma(it[1], it[2], it[3], it[4], it[5])

    def barrier(self):
        toks = []
        for e in self.engs:
            if self.seq[e] > 0:
                toks.append(self._etok(e, self.seq[e] - 1))
        for s in self.dsems:
            if s.cnt > 0:
                toks.append((s, s.cnt, "dma"))
        for e in self.engs:
            self._waits(e, [t for t in toks if not (t[2] == e)])
        for e in ("act", "dve", "pool"):
            if self.seq[e] > 0:
                self._waits(e, [self._etok(e, self.seq[e] - 1)])

    def emit(self):
        nc = self.nc
        streams = self.streams

        def run(eng, lst):
            for it in lst:
                if it[0] == "wait":
                    eng.wait_ge(it[1].h, it[2])
                else:
                    ins = it[1](eng)
                    ins.then_inc(it[2].h, it[3])

        with nc.Block() as block:
            @block.tensor
            def _(eng):
                run(eng, streams["pe"])

            @block.scalar
            def _(eng):
                run(eng, streams["act"])

            @block.vector
            def _(eng):
                run(eng, streams["dve"])

            @block.gpsimd
            def _(eng):
                run(eng, streams["pool"])

            @block.sync
            def _(eng):
                run(eng, streams["sp"])


def _wcols():
    o = {}
    c = 0
    for n, w in [("q", 512), ("kc", 128), ("vc", 128), ("ks", 128), ("vs", 128), ("kw", 128),
                 ("vw", 128), ("gl", 24), ("z", 512), ("xbc", 1024), ("dt", 8)]:
        o[n] = (c, c + w)
        c += w
    return o


WC = _wcols()
W_ORDER = ["q", "kc", "ks", "kw", "vc", "vs", "vw", "gl", "dt", "z", "xbc"]


class Tile:
    def __init__(self, h, name):
        self.h = h
        self.b = Buf(name)

    def __getitem__(self, k):
        return self.h[k]


class View:
    def __init__(self, ap, b):
        self.ap = ap
        self.b = b

    def __getitem__(self, k):
        return self.ap[k]


class Ctx:
    pass


def build(NSEQ, S, phases="ABCDE", dbg=()):
    nc = bass.Bass("TRN2", target_bir_lowering=False)
    es = ExitStack()
    P = Prog(nc, es)
    C = Ctx()
    C.nc, C.es, C.P, C.NSEQ, C.S = nc, es, P, NSEQ, S
    T = NSEQ * S
    NTS = S // 128
    C.T, C.NTS = T, NTS
    C.dbg = dbg

    def din(name, shape, dt=F32):
        return nc.dram_tensor(name, list(shape), dt, kind="ExternalInput").ap()

    def dscr(name, shape, dt):
        kind = "ExternalOutput" if name in dbg else "Internal"
        return nc.dram_tensor(name, list(shape), dt, kind=kind).ap()

    def dout(name, shape, dt=F32):
        return nc.dram_tensor(name, list(shape), dt, kind="ExternalOutput").ap()

    C.din, C.dscr, C.dout = din, dscr, dout
    I = C.I = {}
    I["x"] = din("x", [T, D])
    I["attn_norm_g"] = din("attn_norm_g", [1, D])
    I["w_in"] = din("w_in", [D, INP])
    I["cos"] = din("cos", [128, NTS, 8])
    I["sin"] = din("sin", [128, NTS, 8])
    for kv in "kv":
        I["cmp_pos_" + kv] = din("cmp_pos_" + kv, [32, 64])
        I["cmp_w1_" + kv] = din("cmp_w1_" + kv, [2048, 256])
        I["cmp_b1_" + kv] = din("cmp_b1_" + kv, [256, 1])
        I["cmp_w2_" + kv] = din("cmp_w2_" + kv, [256, 64])
    I["conv_w"] = din("conv_w", [4, 1024])
    I["conv_b"] = din("conv_b", [1024, 1])
    I["dt_bias"] = din("dt_bias", [1, 8])
    I["a_log"] = din("a_log", [1, 8])
    I["d_skip"] = din("d_skip", [1, 8])
    I["ssd_norm_g"] = din("ssd_norm_g", [1, 512])
    I["nsa_norm_g"] = din("nsa_norm_g", [1, 512])
    I["w_out"] = din("w_out", [D, D])
    I["ffn_norm_g"] = din("ffn_norm_g", [1, D])
    I["peer_w_q"] = din("peer_w_q", [D, 2048])
    I["peer_keys"] = din("peer_keys", [16, 128, 128])
    I["peer_u"] = din("peer_u", [16384, D])
    I["peer_v"] = din("peer_v", [16384, D])
    I["final_norm_g"] = din("final_norm_g", [1, D])
    C.out = dout("out", [T, D])

    Sx = C.Sx = {}
    Sx["QT"] = dscr("QT", [NSEQ, NTS, 8, 64, 128], BF16)
    Sx["KT"] = dscr("KT", [4, NSEQ, 128, S], BF16)
    Sx["V"] = dscr("V", [NSEQ, S, 256], BF16)
    Sx["G"] = dscr("G", [T, 24], F32)
    Sx["DT"] = dscr("DT", [T, 8], F32)
    Sx["Z"] = dscr("Z", [T, 512], F32)
    Sx["XBC"] = dscr("XBC", [NSEQ, 1024, S], F32)
    Sx["MIX"] = dscr("MIX", [T, 1024], F32)
    C.dbufs = {}

    def db(*key):
        if key not in C.dbufs:
            C.dbufs[key] = Buf(str(key))
        return C.dbufs[key]

    C.db = db
    C.dbg_out = {}
    if "D" in phases:
        C.UV = dscr("peer_uv_bf", [16384, 2 * D], BF16)
        C.tab = Buf("uv")
        C.tabsems = [P.dsem("tab")] * 2
        for k2, nm in enumerate(("peer_u", "peer_v")):
            for c in range(16):
                P.dma("pool", lambda e, nm=nm, k2=k2, c=c: e.dma_start(out=C.UV[c * 1024:(c + 1) * 1024, k2 * D:(k2 + 1) * D], in_=I[nm][c * 1024:(c + 1) * 1024, :]),
                      C.tabsems[k2], writes=[C.tab])
    if "A" in phases:
        phase_a(C)
    if "B" in phases:
        phase_b(C)
    if "C" in phases:
        phase_c(C)
    if "D" in phases:
        phase_d(C)
    P.barrier()
    P.emit()
    es.close()
    return nc


def mk_tiles(C, es):
    nc = C.nc

    def sb(name, shape, dt=F32):
        return Tile(es.enter_context(nc.sbuf_tensor(name, list(shape), dt)), name)

    def ps(name, shape, dt=F32):
        return Tile(es.enter_context(nc.psum_tensor(name, list(shape), dt)), name)

    return sb, ps


def phase_a(C):
    nc, P, I, Sx, db = C.nc, C.P, C.I, C.Sx, C.db
    NSEQ, S, NTS = C.NSEQ, C.S, C.NTS
    es = ExitStack()
    sb, ps = mk_tiles(C, es)
    W = sb("a_W", [128, 8, INP], BF16)
    gbc = sb("a_gbc", [128, D])
    cos = sb("a_cos", [128, NTS, 8])
    sin = sb("a_sin", [128, NTS, 8])
    ident = sb("a_ident", [128, 128], BF16)
    identf = sb("a_identf", [128, 128])
    junk = sb("a_junk", [128, D])
    NB = 2
    xt = [sb(f"a_x{j}", [128, D]) for j in range(NB)]
    ssq = [sb(f"a_ssq{j}", [128, 1]) for j in range(NB)]
    rstd = [sb(f"a_rstd{j}", [128, 1]) for j in range(NB)]
    hb = [sb(f"a_h{j}", [128, D], BF16) for j in range(NB)]
    hT = [sb(f"a_hT{j}", [128, 8, 128], BF16) for j in range(NB)]
    R = [sb(f"a_R{j}", [128, 1024]) for j in range(NB)]
    RB = [sb(f"a_RB{j}", [128, 1024], BF16) for j in range(NB)]
    tmp = [[sb(f"a_t{j}_{k}", [128, 14, 8]) for k in range(4)] for j in range(NB)]
    qkt = [sb(f"a_qkt{j}", [128, 8, 128], BF16) for j in range(NB)]
    vst = [sb(f"a_vst{j}", [128, 256], BF16) for j in range(NB)]
    gst = [sb(f"a_gst{j}", [128, 24]) for j in range(NB)]
    dtst = [sb(f"a_dtst{j}", [128, 8]) for j in range(NB)]
    zst = [sb(f"a_zst{j}", [128, 512]) for j in range(NB)]
    xbst = [sb(f"a_xbst{j}", [128, 8, 128]) for j in range(NB)]
    pA = ps("a_pA", [128, 512])
    pB = ps("a_pB", [128, 512])
    pC = ps("a_pC", [128, 512])
    pD = ps("a_pD", [128, 512])
    pT = ps("a_pT", [128, 8, 128], BF16)
    pQ = ps("a_pQ", [128, 8, 128], BF16)
    pX = [ps(f"a_pX{j}", [128, 4, 128]) for j in range(2)]

    wv = I["w_in"].rearrange("(c p) n -> p c n", p=128)
    for c in range(8):
        P.dma("pool", lambda e, c=c: e.dma_start(out=W[:, c, :], in_=wv[:, c, :]), P.S("W"), writes=[W.b])
    P.dma("sp", lambda e: e.dma_start(out=gbc[:], in_=I["attn_norm_g"].partition_broadcast(128)), P.S("gbc"), writes=[gbc.b])
    P.dma("sp", lambda e: e.dma_start(out=cos[:], in_=I["cos"]), P.S("cos"), writes=[cos.b])
    P.dma("sp", lambda e: e.dma_start(out=sin[:], in_=I["sin"]), P.S("sin"), writes=[sin.b])
    make_ident(C, ident, identf)

    cA, cB, cC, cD, cE = 0, 512, 1024, 1312, 1824
    for ti in range(NSEQ * NTS):
        j = ti % NB
        seq, i = divmod(ti, NTS)
        r0 = ti * 128
        P.dma("sp", lambda e, j=j, r0=r0: e.dma_start(out=xt[j][:], in_=I["x"][r0:r0 + 128, :]), P.S(("x", j)), writes=[xt[j].b])
        P.op("act", lambda e, j=j: e.activation(out=junk[:], in_=xt[j][:], func=AF.Square, accum_out=ssq[j][:]),
             reads=[xt[j].b], writes=[ssq[j].b])
        P.op("act", lambda e, j=j: e.activation(out=rstd[j][:], in_=ssq[j][:], func=AF.Sqrt, scale=1.0 / D, bias=EPS),
             reads=[ssq[j].b], writes=[rstd[j].b])
        P.op("dve", lambda e, j=j: e.reciprocal(out=rstd[j][:], in_=rstd[j][:]), reads=[rstd[j].b], writes=[rstd[j].b])
        P.op("dve", lambda e, j=j: e.scalar_tensor_tensor(out=hb[j][:], in0=xt[j][:], scalar=rstd[j][:, 0:1], in1=gbc[:],
                                                           op0=ALU.mult, op1=ALU.mult),
             reads=[xt[j].b, rstd[j].b, gbc.b], writes=[hb[j].b])
        for c in range(8):
            P.op("pe", lambda e, j=j, c=c: e.transpose(out=pT[:, c, :], in_=hb[j][:, c * 128:(c + 1) * 128], identity=ident[:]),
                 reads=[hb[j].b, ident.b], writes=[pT.b])
        P.op("act", lambda e, j=j: e.activation(out=hT[j][:], in_=pT[:], func=AF.Copy), reads=[pT.b], writes=[hT[j].b])
        for (pt, c0, c1) in ((pA, cA, cB), (pB, cB, cC), (pC, cC, cD), (pD, cD, cE)):
            for c in range(8):
                P.op("pe", lambda e, j=j, c=c, pt=pt, c0=c0, c1=c1: e.matmul(
                    pt[:, 0:c1 - c0], lhsT=hT[j][:, c, :], rhs=W[:, c, c0:c1], start=(c == 0), stop=(c == 7)),
                    reads=[hT[j].b, W.b], writes=[pt.b])
        for cc in range(8):
            for c in range(8):
                P.op("pe", lambda e, j=j, c=c, cc=cc: e.matmul(
                    pX[cc // 4][:, cc % 4, :], lhsT=W[:, c, cE + cc * 128:cE + (cc + 1) * 128], rhs=hT[j][:, c, :],
                    start=(c == 0), stop=(c == 7)), reads=[hT[j].b, W.b], writes=[pX[cc // 4].b])
        P.op("act", lambda e, j=j: e.activation(out=R[j][:, 0:512], in_=pA[:], func=AF.Copy, scale=0.125),
             reads=[pA.b], writes=[R[j].b])
        P.op("dve", lambda e, j=j: e.tensor_copy(out=R[j][:, 512:1024], in_=pB[:]), reads=[pB.b, R[j].b], writes=[R[j].b])
        P.op("act", lambda e, j=j: e.activation(out=RB[j][:], in_=R[j][:], func=AF.Copy), reads=[R[j].b], writes=[RB[j].b])
        R3 = R[j][:, 0:896].rearrange("p (h d) -> p h d", d=64)
        RB3 = RB[j][:, 0:896].rearrange("p (h d) -> p h d", d=64)
        cb_ = cos[:, i, :].unsqueeze(1).to_broadcast([128, 14, 8])
        sb_ = sin[:, i, :].unsqueeze(1).to_broadcast([128, 14, 8])
        t0, t1, t2, t3 = tmp[j]
        x1, x2 = R3[:, :, 0:8], R3[:, :, 8:16]
        P.op("dve", lambda e, t0=t0, x1=x1, cb_=cb_: e.tensor_mul(out=t0[:], in0=x1, in1=cb_), reads=[R[j].b, cos.b], writes=[t0.b])
        P.op("dve", lambda e, t1=t1, x2=x2, sb_=sb_: e.tensor_mul(out=t1[:], in0=x2, in1=sb_), reads=[R[j].b, sin.b], writes=[t1.b])
        P.op("dve", lambda e, t2=t2, x2=x2, cb_=cb_: e.tensor_mul(out=t2[:], in0=x2, in1=cb_), reads=[R[j].b, cos.b], writes=[t2.b])
        P.op("dve", lambda e, t3=t3, x1=x1, sb_=sb_: e.tensor_mul(out=t3[:], in0=x1, in1=sb_), reads=[R[j].b, sin.b], writes=[t3.b])
        P.op("dve", lambda e, RB3=RB3, t0=t0, t1=t1: e.tensor_sub(out=RB3[:, :, 0:8], in0=t0[:], in1=t1[:]),
             reads=[t0.b, t1.b, RB[j].b], writes=[RB[j].b])
        P.op("dve", lambda e, RB3=RB3, t2=t2, t3=t3: e.tensor_add(out=RB3[:, :, 8:16], in0=t2[:], in1=t3[:]),
             reads=[t2.b, t3.b, RB[j].b], writes=[RB[j].b])
        for m in range(8):
            P.op("pe", lambda e, j=j, m=m: e.transpose(out=pQ[:, m, :], in_=RB[j][:, m * 128:(m + 1) * 128], identity=ident[:]),
                 reads=[RB[j].b, ident.b], writes=[pQ.b])
        P.op("dve", lambda e, j=j: e.tensor_copy(out=qkt[j][:], in_=pQ[:]), reads=[pQ.b], writes=[qkt[j].b])
        P.op("act", lambda e, j=j: e.activation(out=vst[j][:], in_=pC[:, 0:256], func=AF.Copy), reads=[pC.b], writes=[vst[j].b])
        P.op("act", lambda e, j=j: e.activation(out=gst[j][:], in_=pC[:, 256:280], func=AF.Sigmoid), reads=[pC.b], writes=[gst[j].b])
        P.op("act", lambda e, j=j: e.activation(out=dtst[j][:], in_=pC[:, 280:288], func=AF.Copy), reads=[pC.b], writes=[dtst[j].b])
        P.op("act", lambda e, j=j: e.activation(out=zst[j][:], in_=pD[:], func=AF.Silu), reads=[pD.b], writes=[zst[j].b])
        P.op("dve", lambda e, j=j: e.tensor_copy(out=xbst[j][:, 0:4, :], in_=pX[0][:]), reads=[pX[0].b], writes=[xbst[j].b])
        P.op("dve", lambda e, j=j: e.tensor_copy(out=xbst[j][:, 4:8, :], in_=pX[1][:]), reads=[pX[1].b, xbst[j].b], writes=[xbst[j].b])
        for half in range(2):
            dst = Sx["QT"][seq, i, half::2, :, :].rearrange("m d t -> d m t")
            src = qkt[j][half * 64:(half + 1) * 64, 0:4, :]
            P.dma("pool", lambda e, dst=dst, src=src: e.dma_start(out=dst, in_=src), P.S(("qt", j, half)), reads=[qkt[j].b], writes=[db("QT", seq, i)])
        dst = Sx["KT"][:, seq, :, i * 128:(i + 1) * 128].rearrange("k p t -> p k t")
        P.dma("pool", lambda e, dst=dst, j=j: e.dma_start(out=dst, in_=qkt[j][:, 4:8, :]), P.S(("kt", j)), reads=[qkt[j].b], writes=[db("KT", seq, i)])
        P.dma("pool", lambda e, j=j, seq=seq, i=i: e.dma_start(out=Sx["V"][seq, i * 128:(i + 1) * 128, :], in_=vst[j][:]), P.S(("v", j)),
              reads=[vst[j].b], writes=[db("V", seq, i)])
        P.dma("pool", lambda e, j=j, r0=r0: e.dma_start(out=Sx["G"][r0:r0 + 128, :], in_=gst[j][:]), P.S(("g", j)), reads=[gst[j].b], writes=[db("G", ti)])
        P.dma("pool", lambda e, j=j, r0=r0: e.dma_start(out=Sx["DT"][r0:r0 + 128, :], in_=dtst[j][:]), P.S(("dt", j)), reads=[dtst[j].b], writes=[db("DT", ti)])
        P.dma("pool", lambda e, j=j, r0=r0: e.dma_start(out=Sx["Z"][r0:r0 + 128, :], in_=zst[j][:]), P.S(("z", j)), reads=[zst[j].b], writes=[db("Z", ti)])
        dst = Sx["XBC"][seq, :, i * 128:(i + 1) * 128].rearrange("(c p) t -> p c t", p=128)
        P.dma("pool", lambda e, dst=dst, j=j: e.dma_start(out=dst, in_=xbst[j][:]), P.S(("xbc", j)), reads=[xbst[j].b], writes=[db("XBC", seq, i)])
    P.end_phase()
    es.close()


def make_ident(C, ident, identf):
    nc, P = C.nc, C.P
    P.op("pool", lambda e: e.memset(identf[:], 1.0), writes=[identf.b])
    P.op("pool", lambda e: e.affine_select(out=identf[:], in_=identf[:], pattern=[[1, 128]], compare_op=ALU.is_equal,
                                            fill=0.0, base=0, channel_multiplier=-1), reads=[identf.b], writes=[identf.b])
    P.op("dve", lambda e: e.tensor_copy(out=ident[:], in_=identf[:]), reads=[identf.b], writes=[ident.b])


def host_maps(inputs, NSEQ, S, ncores):
    f = lambda a: np.ascontiguousarray(np.asarray(a, dtype=np.float32))
    x = f(inputs["x"])
    NTS = S // 128
    w_in = f(inputs["w_in"])[0]
    w_re = np.concatenate([w_in[:, WC[n][0]:WC[n][1]] for n in W_ORDER], axis=1)
    pos = (np.arange(NTS)[None, :] * 128 + np.arange(128)[:, None]).astype(np.float32)
    inv = np.power(np.float32(500000.0), -np.arange(8, dtype=np.float32) * 2.0 / 16).astype(np.float32)
    ang = pos[:, :, None] * inv[None, None, :]
    shared = {
        "attn_norm_g": f(inputs["attn_norm_g"]).reshape(1, D),
        "w_in": np.ascontiguousarray(w_re),
        "cos": np.cos(ang).astype(np.float32), "sin": np.sin(ang).astype(np.float32),
        "conv_w": f(inputs["conv_w"])[0], "conv_b": f(inputs["conv_b"]).reshape(1024, 1),
        "dt_bias": f(inputs["dt_bias"]).reshape(1, 8), "a_log": f(inputs["a_log"]).reshape(1, 8),
        "d_skip": f(inputs["d_skip"]).reshape(1, 8),
        "ssd_norm_g": f(inputs["ssd_norm_g"]).reshape(1, 512), "nsa_norm_g": f(inputs["nsa_norm_g"]).reshape(1, 512),
        "w_out": f(inputs["w_out"])[0], "ffn_norm_g": f(inputs["ffn_norm_g"]).reshape(1, D),
        "peer_w_q": f(inputs["peer_w_q"])[0], "peer_keys": f(inputs["peer_keys"]).reshape(16, 128, 128),
        "peer_u": f(inputs["peer_u"])[0], "peer_v": f(inputs["peer_v"])[0],
        "final_norm_g": f(inputs["final_norm_g"]).reshape(1, D),
    }
    for kv in "kv":
        shared["cmp_pos_" + kv] = f(inputs["cmp_pos_" + kv])[0]
        shared["cmp_w1_" + kv] = f(inputs["cmp_w1_" + kv])[0]
        shared["cmp_b1_" + kv] = f(inputs["cmp_b1_" + kv]).reshape(256, 1)
        shared["cmp_w2_" + kv] = f(inputs["cmp_w2_" + kv])[0]
    maps = []
    for c in range(ncores):
        m = dict(shared)
        m["x"] = np.ascontiguousarray(x[c * NSEQ:(c + 1) * NSEQ].reshape(NSEQ * S, D))
        maps.append(m)
    return maps


_NC_CACHE = {}


def kernel(**inputs):
    x = np.asarray(inputs["x"])
    B, S, _ = x.shape
    ncores = 8
    NSEQ = B // ncores
    key = (NSEQ, S)
    if key not in _NC_CACHE:
        _NC_CACHE[key] = build(NSEQ, S)
    nc = _NC_CACHE[key]
    maps = host_maps(inputs, NSEQ, S, ncores)
    res = run_bass_kernel_spmd(nc, maps, core_ids=list(range(ncores)))
    out = np.concatenate([np.asarray(r["out"]).reshape(NSEQ, S, D) for r in res.results], axis=0)
    return out.astype(np.float32)


def phase_b(C):
    nc, P, I, Sx, db = C.nc, C.P, C.I, C.Sx, C.db
    NSEQ, S, NTS = C.NSEQ, C.S, C.NTS
    NCMP = S // 16 - 1
    NSEL = S // 64
    es = ExitStack()
    sb, ps = mk_tiles(C, es)
    Sx["ONSA"] = C.dscr("ONSA", [C.T, 512], F32)
    ident = sb("b_ident", [128, 128], BF16)
    identf = sb("b_identf", [128, 128])
    w1 = {kv: sb("b_w1" + kv, [64, 32, 256], BF16) for kv in "kv"}
    w2 = {kv: sb("b_w2" + kv, [128, 2, 64], BF16) for kv in "kv"}
    posT = {kv: sb("b_pos" + kv, [64, 32], BF16) for kv in "kv"}
    b1 = {kv: sb("b_b1" + kv, [128, 2]) for kv in "kv"}
    b1t = {kv: sb("b_b1t" + kv, [128, 2]) for kv in "kv"}
    Kst = sb("b_Kst", [128, S], BF16)
    kwT = sb("b_kwT", [64, S], BF16)
    kcT = sb("b_kcT", [64, S], BF16)
    vcT = sb("b_vcT", [64, S], BF16)
    Vs = sb("b_Vs", [128, NTS, 65], BF16)
    Vw = sb("b_Vw", [128, NTS, 65], BF16)
    cm = sb("b_cm", [128, 8])
    cmaskT = sb("b_cmaskT", [128, 128], BF16)
    wmaskT = sb("b_wmaskT", [128, 128], BF16)
    A3 = sb("b_A3", [128, 3])
    M3 = sb("b_M3", [128, 3])
    zrow = sb("b_zrow", [1, 128], BF16)
    zrhs = sb("b_zrhs", [1, 512], BF16)
    hu = sb("b_hu", [128, 256])
    hw = sb("b_hw", [128, 256])
    hs = sb("b_hs", [128, 256])
    hidT = [sb(f"b_hidT{c}", [128, 256], BF16) for c in range(2)]
    kcmpT = sb("b_kcmpT", [64, 256], BF16)
    vcmp = sb("b_vcmp", [128, 2, 64], BF16)
    NB = 2
    Qst = [sb(f"b_Qst{j}", [128, 512], BF16) for j in range(NB)]
    gts = [sb(f"b_g{j}", [128, 24]) for j in range(NB)]
    sc = sb("b_sc", [128, 4, 256])
    pe_ = sb("b_pe", [128, 4, 256])
    pn = sb("b_pn", [128, 4, 256])
    zs = sb("b_zs", [128, 4])
    rz = sb("b_rz", [128, 4])
    PPW = 4 * NSEL + 4
    PP = sb("b_PP", [128, PPW])
    imp = sb("b_imp", [128, 64])
    imp2 = sb("b_imp2", [128, 64])
    m8a = sb("b_m8a", [128, 8])
    m8b = sb("b_m8b", [128, 8])
    biasm = sb("b_biasm", [128, 128])
    ptb = sb("b_ptb", [128, 2, 128], BF16)
    pts = [sb(f"b_pts{j}", [128, 512], BF16) for j in range(3)]
    ptw = [sb(f"b_ptw{j}", [128, 512], BF16) for j in range(3)]
    osb = sb("b_osb", [128, 4, 65])
    owb = sb("b_owb", [128, 4, 65])
    rsel = sb("b_rsel", [128, 4])
    rwin = sb("b_rwin", [128, 4])
    wsel = sb("b_wsel", [128, 4])
    wwin = sb("b_wwin", [128, 4])
    ot = [sb(f"b_ot{j}", [128, 4, 64]) for j in range(NB)]
    t2 = sb("b_t2", [128, 4, 64])
    pc = [ps(f"b_pc{j}", [128, 512]) for j in range(2)]
    pmisc = ps("b_pmisc", [128, 512])
    poc = ps("b_poc", [128, 4, 64])
    pss = [ps(f"b_pss{j}", [128, 512]) for j in range(2)]
    posel = ps("b_posel", [128, 512])
    powin = ps("b_powin", [128, 512])

    make_ident(C, ident, identf)
    for kv in "kv":
        w1v = I["cmp_w1_" + kv].rearrange("(p d) h -> d p h", d=64)
        for q4 in range(4):
            P.dma("pool", lambda e, kv=kv, q4=q4, w1v=w1v: e.dma_start(out=w1[kv][:, q4 * 8:(q4 + 1) * 8, :], in_=w1v[:, q4 * 8:(q4 + 1) * 8, :]),
                  P.S(("w1", kv)), writes=[w1[kv].b])
        P.dma("pool", lambda e, kv=kv: e.dma_start(out=w2[kv][:], in_=I["cmp_w2_" + kv].rearrange("(c p) d -> p c d", p=128)),
              P.S(("w2", kv)), writes=[w2[kv].b])
        P.dma("pool", lambda e, kv=kv: e.dma_start(out=posT[kv][:], in_=I["cmp_pos_" + kv].rearrange("p d -> d p"),
                                                    allow_slow_non_contiguous=True), P.S(("pos", kv)), writes=[posT[kv].b])
        P.dma("sp", lambda e, kv=kv: e.dma_start(out=b1[kv][:], in_=I["cmp_b1_" + kv].rearrange("(c p) o -> p (c o)", p=128),
                                                  allow_slow_non_contiguous=True), P.S(("b1", kv)), writes=[b1[kv].b])
        for hc in range(2):
            for p in range(32):
                P.op("pe", lambda e, kv=kv, hc=hc, p=p: e.matmul(pmisc[:, hc:hc + 1], lhsT=w1[kv][:, p, hc * 128:(hc + 1) * 128],
                                                                  rhs=posT[kv][:, p:p + 1], start=(p == 0), stop=(p == 31)),
                     reads=[w1[kv].b, posT[kv].b], writes=[pmisc.b])
        P.op("dve", lambda e, kv=kv: e.tensor_add(out=b1t[kv][:], in0=pmisc[:, 0:2], in1=b1[kv][:]),
             reads=[pmisc.b, b1[kv].b], writes=[b1t[kv].b])
    P.op("pool", lambda e: e.memset(Kst[64:128, :], 1.0), writes=[Kst.b])
    P.op("pool", lambda e: e.affine_select(out=Kst[64:128, :], in_=Kst[64:128, :], pattern=[[1, S]], compare_op=ALU.is_ge,
                                            fill=0.0, base=0, channel_multiplier=-64), reads=[Kst.b], writes=[Kst.b])
    P.op("pool", lambda e: e.affine_select(out=Kst[64:128, :], in_=Kst[64:128, :], pattern=[[-1, S]], compare_op=ALU.is_ge,
                                            fill=0.0, base=63, channel_multiplier=64), reads=[Kst.b], writes=[Kst.b])
    P.op("pool", lambda e: e.memset(cm[:], 0.0), writes=[cm.b])
    P.op("pool", lambda e: e.affine_select(out=cm[:], in_=cm[:], pattern=[[-16, 8]], compare_op=ALU.is_ge,
                                            fill=NEG, base=-15, channel_multiplier=1), reads=[cm.b], writes=[cm.b])
    P.op("pool", lambda e: e.memset(cmaskT[:], 1.0), writes=[cmaskT.b])
    P.op("pool", lambda e: e.affine_select(out=cmaskT[:], in_=cmaskT[:], pattern=[[1, 128]], compare_op=ALU.is_ge,
                                            fill=0.0, base=0, channel_multiplier=-1), reads=[cmaskT.b], writes=[cmaskT.b])
    P.op("pool", lambda e: e.memset(wmaskT[:], 1.0), writes=[wmaskT.b])
    P.op("pool", lambda e: e.affine_select(out=wmaskT[:], in_=wmaskT[:], pattern=[[-1, 128]], compare_op=ALU.is_gt,
                                            fill=0.0, base=0, channel_multiplier=1), reads=[wmaskT.b], writes=[wmaskT.b])
    P.op("dve", lambda e: e.memset(A3[:], 1e9), writes=[A3.b])
    P.op("dve", lambda e: e.memset(A3[64:128, 0:1], 0.0), reads=[A3.b], writes=[A3.b])
    P.op("dve", lambda e: e.memset(A3[0:64, 2:3], -1.0), reads=[A3.b], writes=[A3.b])
    P.op("dve", lambda e: e.memset(M3[:], 0.0), writes=[M3.b])
    P.op("dve", lambda e: e.memset(M3[64:128, 0:1], 1.0), reads=[M3.b], writes=[M3.b])
    P.op("dve", lambda e: e.memset(zrow[:], 0.0), writes=[zrow.b])
    P.op("dve", lambda e: e.memset(zrhs[:], 0.0), writes=[zrhs.b])
    P.op("dve", lambda e: e.memset(biasm[:], 0.0), writes=[biasm.b])
    P.op("dve", lambda e: e.memset(Vs[:], 1.0), writes=[Vs.b])
    P.op("dve", lambda e: e.memset(Vw[:], 1.0), writes=[Vw.b])

    cnt = 0
    for seq in range(NSEQ):
        for g in range(2):
            kbufs = [db("KT", seq, i) for i in range(NTS)]
            vbufs = [db("V", seq, i) for i in range(NTS)]
            rows = slice(g * 64, (g + 1) * 64)
            P.dma("sp", lambda e, seq=seq, rows=rows: e.dma_start(out=kcT[:], in_=Sx["KT"][0, seq, rows, :]), P.S("kc"), reads=kbufs, writes=[kcT.b])
            P.dma("sp", lambda e, seq=seq, rows=rows: e.dma_start(out=Kst[0:64, :], in_=Sx["KT"][1, seq, rows, :]), P.S("ks"), reads=kbufs, writes=[Kst.b])
            P.dma("sp", lambda e, seq=seq, rows=rows: e.dma_start(out=kwT[:], in_=Sx["KT"][2, seq, rows, :]), P.S("kw"), reads=kbufs, writes=[kwT.b])
            P.dma("sp", lambda e, seq=seq, rows=rows: e.dma_start(out=vcT[:], in_=Sx["KT"][3, seq, rows, :]), P.S("vc"), reads=kbufs, writes=[vcT.b])
            P.dma("sp", lambda e, seq=seq, g=g: e.dma_start(out=Vs[:, :, 0:64], in_=Sx["V"][seq, :, g * 64:(g + 1) * 64].rearrange("(i p) d -> p i d", p=128)),
                  P.S("vs"), reads=vbufs, writes=[Vs.b])
            P.dma("sp", lambda e, seq=seq, g=g: e.dma_start(out=Vw[:, :, 0:64], in_=Sx["V"][seq, :, 128 + g * 64:128 + (g + 1) * 64].rearrange("(i p) d -> p i d", p=128)),
                  P.S("vw"), reads=vbufs, writes=[Vw.b])
            for kv, src in (("k", kcT), ("v", vcT)):
                for hc in range(2):
                    hp = pss[hc]
                    for p in range(32):
                        P.op("pe", lambda e, kv=kv, hc=hc, p=p, src=src, hp=hp: e.matmul(
                            hp[:, 0:NCMP], lhsT=w1[kv][:, p, hc * 128:(hc + 1) * 128], rhs=src[:, p:p + 16 * (NCMP - 1) + 1:16],
                            start=(p == 0), stop=(p == 31)), reads=[w1[kv].b, src.b], writes=[hp.b])
                    P.op("act", lambda e, kv=kv, hc=hc, hp=hp: e.activation(out=hu[:, 0:NCMP], in_=hp[:, 0:NCMP], func=AF.Identity,
                                                                          bias=b1t[kv][:, hc:hc + 1]), reads=[hp.b, b1t[kv].b], writes=[hu.b])
                    P.op("dve", lambda e: e.tensor_mul(out=hw[:, 0:NCMP], in0=hu[:, 0:NCMP], in1=hu[:, 0:NCMP]), reads=[hu.b], writes=[hw.b])
                    P.op("dve", lambda e: e.tensor_scalar(out=hw[:, 0:NCMP], in0=hw[:, 0:NCMP], scalar1=0.044715, scalar2=1.0,
                                                          op0=ALU.mult, op1=ALU.add), reads=[hw.b], writes=[hw.b])
                    P.op("dve", lambda e: e.tensor_mul(out=hw[:, 0:NCMP], in0=hw[:, 0:NCMP], in1=hu[:, 0:NCMP]), reads=[hw.b, hu.b], writes=[hw.b])
                    P.op("act", lambda e: e.activation(out=hs[:, 0:NCMP], in_=hw[:, 0:NCMP], func=AF.Sigmoid, scale=1.5957691216057308),
                         reads=[hw.b], writes=[hs.b])
                    P.op("dve", lambda e, hc=hc: e.tensor_mul(out=hidT[hc][:, 0:NCMP], in0=hu[:, 0:NCMP], in1=hs[:, 0:NCMP]),
                         reads=[hu.b, hs.b], writes=[hidT[hc].b])
                if kv == "k":
                    for hc in range(2):
                        P.op("pe", lambda e, hc=hc: e.matmul(pc[0][0:64, 0:NCMP], lhsT=w2["k"][:, hc, :], rhs=hidT[hc][:, 0:NCMP],
                                                             start=(hc == 0), stop=(hc == 1)), reads=[w2["k"].b, hidT[hc].b], writes=[pc[0].b])
                    P.op("act", lambda e: e.activation(out=kcmpT[:, 0:NCMP], in_=pc[0][0:64, 0:NCMP], func=AF.Copy), reads=[pc[0].b], writes=[kcmpT.b])
                else:
                    for jc in range(2):
                        nj = min(128, NCMP - jc * 128)
                        if nj <= 0:
                            continue
                        for hc in range(2):
                            P.op("pe", lambda e, hc=hc, jc=jc, nj=nj: e.matmul(pc[1][0:nj, jc * 64:(jc + 1) * 64], lhsT=hidT[hc][:, jc * 128:jc * 128 + nj],
                                                                           rhs=w2["v"][:, hc, :], start=(hc == 0), stop=(hc == 1)),
                                 reads=[w2["v"].b, hidT[hc].b], writes=[pc[1].b])
                        P.op("act", lambda e, jc=jc, nj=nj: e.activation(out=vcmp[0:nj, jc, :], in_=pc[1][0:nj, jc * 64:(jc + 1) * 64], func=AF.Copy),
                             reads=[pc[1].b], writes=[vcmp.b])
            for i in range(NTS):
                j = cnt % NB
                cnt += 1
                ti = seq * NTS + i
                r0 = ti * 128
                Q = Qst[j]
                P.dma("sp", lambda e, Q=Q, seq=seq, i=i, g=g: e.dma_start(
                    out=Q[0:64, :].rearrange("d (r t) -> d r t", r=4), in_=Sx["QT"][seq, i, 4 * g:4 * g + 4, :, :].rearrange("r d t -> d r t")),
                    P.S(("q", j)), reads=[db("QT", seq, i)], writes=[Q.b])
                P.dma("sp", lambda e, j=j, r0=r0: e.dma_start(out=gts[j][:], in_=Sx["G"][r0:r0 + 128, :]), P.S(("gt", j)), reads=[db("G", ti)], writes=[gts[j].b])
                def branch(nm, pacc, ptl, lhs_of, rows_k, Vt, kts, kb, Q=Q, i=i):
                    P.op("pe", lambda e, pacc=pacc: e.matmul(pacc[:, 0:260], lhsT=zrow[:], rhs=zrhs[:, 0:260], start=True, stop=False, skip_group_check=True),
                         reads=[zrow.b, zrhs.b], writes=[pacc.b])
                    nk = len(kts)
                    for n_ in range(nk + 1):
                        if n_ < nk:
                            kt = kts[n_]
                            pb = pss[n_ % 2]
                            pt_ = ptl[n_ % 3]
                            P.op("pe", lambda e, pb=pb, kt=kt: e.matmul(pb[:], lhsT=lhs_of(kt), rhs=Q[0:rows_k, :], start=True, stop=True),
                                 reads=[kb, Q.b], writes=[pb.b])
                            P.op("act", lambda e, pb=pb, pt_=pt_: e.activation(out=pt_[:], in_=pb[:], func=AF.Exp), reads=[pb.b], writes=[pt_.b])
                            msk = None
                            if kt == i:
                                msk = cmaskT
                            elif nm == "w" and kt == i - 4:
                                msk = wmaskT
                            if msk is not None:
                                P.op("dve", lambda e, pt_=pt_, msk=msk: e.tensor_mul(out=pt_[:].rearrange("p (r t) -> p r t", r=4), in0=pt_[:].rearrange("p (r t) -> p r t", r=4),
                                                                                    in1=msk[:].unsqueeze(1).to_broadcast([128, 4, 128])),
                                     reads=[pt_.b, msk.b], writes=[pt_.b])
                        if n_ >= 1:
                            kt = kts[n_ - 1]
                            pt_ = ptl[(n_ - 1) % 3]
                            for r in range(4):
                                P.op("pe", lambda e, pacc=pacc, pt_=pt_, r=r, kt=kt, last=(n_ == nk): e.matmul(
                                    pacc[:, r * 65:(r + 1) * 65], lhsT=pt_[:, r * 128:(r + 1) * 128], rhs=Vt[:, kt, :], start=False, stop=last, skip_group_check=True),
                                    reads=[pt_.b, Vt.b], writes=[pacc.b])
                ncols = min(8 * i + 7, NCMP)
                for r in range(4):
                    P.op("pe", lambda e, Q=Q, r=r, ncols=ncols: e.matmul(pc[r // 2][:, (r % 2) * 256:(r % 2) * 256 + ncols], lhsT=Q[0:64, r * 128:(r + 1) * 128],
                                                                         rhs=kcmpT[:, 0:ncols], start=True, stop=True),
                         reads=[Q.b, kcmpT.b], writes=[pc[r // 2].b])
                for h2 in range(2):
                    P.op("act", lambda e, h2=h2, ncols=ncols: e.activation(out=sc[:, 2 * h2:2 * h2 + 2, 0:ncols],
                                                                          in_=pc[h2][:].rearrange("p (r c) -> p r c", r=2)[:, :, 0:ncols], func=AF.Copy),
                         reads=[pc[h2].b, sc.b], writes=[sc.b])
                lo = max(8 * i - 1, 0)
                hi = ncols
                a = lo - (8 * i - 1)
                P.op("dve", lambda e, lo=lo, hi=hi, a=a: e.tensor_add(out=sc[:, :, lo:hi], in0=sc[:, :, lo:hi],
                                                                     in1=cm[:, a:a + hi - lo].unsqueeze(1).to_broadcast([128, 4, hi - lo])),
                     reads=[sc.b, cm.b], writes=[sc.b])
                for r in range(4):
                    P.op("act", lambda e, r=r, ncols=ncols: e.activation(out=pe_[:, r, 0:ncols], in_=sc[:, r, 0:ncols], func=AF.Exp, accum_out=zs[:, r:r + 1]),
                         reads=[sc.b, pe_.b, zs.b], writes=[pe_.b, zs.b])
                P.op("dve", lambda e: e.tensor_scalar_add(out=rz[:], in0=zs[:], scalar1=1e-30), reads=[zs.b], writes=[rz.b])
                P.op("dve", lambda e: e.reciprocal(out=rz[:], in_=rz[:]), reads=[rz.b], writes=[rz.b])
                P.op("dve", lambda e, ncols=ncols: e.tensor_mul(out=pn[:, :, 0:ncols], in0=pe_[:, :, 0:ncols],
                                                                in1=rz[:].unsqueeze(2).to_broadcast([128, 4, ncols])), reads=[pe_.b, rz.b], writes=[pn.b])
                if 2 * i + 1 > 15:
                    P.op("dve", lambda e: e.memset(PP[:], 0.0), writes=[PP.b])
                    P.op("dve", lambda e, ncols=ncols: e.tensor_add(out=PP[:, 1:1 + ncols], in0=pn[:, 0, 0:ncols], in1=pn[:, 1, 0:ncols]), reads=[pn.b, PP.b], writes=[PP.b])
                    for r in (2, 3):
                        P.op("dve", lambda e, ncols=ncols, r=r: e.tensor_add(out=PP[:, 1:1 + ncols], in0=PP[:, 1:1 + ncols], in1=pn[:, r, 0:ncols]), reads=[pn.b, PP.b], writes=[PP.b])
                    P.op("dve", lambda e: e.tensor_reduce(out=imp[:, 0:NSEL], in_=PP[:, 0:4 * NSEL].rearrange("p (b m) -> p b m", m=4), axis=AX.X, op=ALU.add),
                         reads=[PP.b], writes=[imp.b])
                    P.op("dve", lambda e: e.tensor_add(out=imp[:, 0:NSEL], in0=imp[:, 0:NSEL], in1=PP[:, 4:4 * NSEL + 4:4]), reads=[PP.b, imp.b], writes=[imp.b])
                    if 2 * i + 2 < 64:
                        P.op("dve", lambda e, i=i: e.memset(imp[:, 2 * i + 2:64], -1.0), reads=[imp.b], writes=[imp.b])
                    P.op("dve", lambda e, i=i: e.tensor_mul(out=imp[:, 2 * i - 1:2 * i + 2], in0=imp[:, 2 * i - 1:2 * i + 2], in1=M3[:]), reads=[imp.b, M3.b], writes=[imp.b])
                    P.op("dve", lambda e, i=i: e.tensor_add(out=imp[:, 2 * i - 1:2 * i + 2], in0=imp[:, 2 * i - 1:2 * i + 2], in1=A3[:]), reads=[imp.b, A3.b], writes=[imp.b])
                    P.op("dve", lambda e: e.memset(imp[:, 0:1], 1e9), reads=[imp.b], writes=[imp.b])
                    P.op("dve", lambda e: e.max(out=m8a[:], in_=imp[:]), reads=[imp.b], writes=[m8a.b])
                    P.op("dve", lambda e: e.match_replace(out=imp2[:], in_to_replace=m8a[:], in_values=imp[:], imm_value=-2.0), reads=[imp.b, m8a.b], writes=[imp2.b])
                    P.op("dve", lambda e: e.max(out=m8b[:], in_=imp2[:]), reads=[imp2.b], writes=[m8b.b])
                    P.op("dve", lambda e: e.tensor_scalar(out=biasm[:, 64:128], in0=imp[:], scalar1=m8b[:, 7:8], scalar2=NEG, op0=ALU.is_lt, op1=ALU.mult),
                         reads=[imp.b, m8b.b, biasm.b], writes=[biasm.b])
                    pass
                branch("w", powin, ptw, lambda kt: kwT[:, kt * 128:(kt + 1) * 128], 64, Vw, list(range(max(0, i - 4), i + 1)), kwT.b)
                njc = (ncols + 127) // 128
                for r in range(4):
                    for jc in range(njc):
                        nj = min(128, ncols - jc * 128)
                        P.op("pe", lambda e, r=r, jc=jc, nj=nj: e.transpose(out=pmisc[0:nj, jc * 128:(jc + 1) * 128], in_=pn[:, r, jc * 128:jc * 128 + nj],
                                                                          identity=identf[:]), reads=[pn.b, identf.b], writes=[pmisc.b])
                        P.op("act", lambda e, jc=jc, nj=nj: e.activation(out=ptb[0:nj, jc, :], in_=pmisc[0:nj, jc * 128:(jc + 1) * 128], func=AF.Copy),
                             reads=[pmisc.b, ptb.b], writes=[ptb.b])
                    for jc in range(njc):
                        nj = min(128, ncols - jc * 128)
                        P.op("pe", lambda e, r=r, jc=jc, nj=nj, njc=njc: e.matmul(poc[:, r, :], lhsT=ptb[0:nj, jc, :], rhs=vcmp[0:nj, jc, :],
                                                                                 start=(jc == 0), stop=(jc == njc - 1)),
                             reads=[ptb.b, vcmp.b], writes=[poc.b])
                if 2 * i + 1 > 15:
                    P.op("pe", lambda e: e.transpose(out=pmisc[:, 256:384], in_=biasm[:], identity=identf[:]), reads=[biasm.b, identf.b], writes=[pmisc.b])
                    P.op("act", lambda e, Q=Q: e.activation(out=Q[64:128, :].rearrange("p (r t) -> p r t", r=4),
                                                           in_=pmisc[64:128, 256:384].unsqueeze(1).to_broadcast([64, 4, 128]), func=AF.Copy),
                         reads=[pmisc.b, Q.b], writes=[Q.b])
                else:
                    P.op("dve", lambda e, Q=Q: e.memset(Q[64:128, :], 0.0), reads=[Q.b], writes=[Q.b])
                branch("s", posel, pts, lambda kt: Kst[:, kt * 128:(kt + 1) * 128], 128, Vs, list(range(0, i + 1)), Kst.b)
                gv = gts[j][:, g * 12:(g + 1) * 12].rearrange("p (r k) -> p r k", k=3)
                P.op("act", lambda e: e.activation(out=osb[:], in_=posel[:, 0:260].rearrange("p (r c) -> p r c", c=65), func=AF.Copy), reads=[posel.b], writes=[osb.b])
                P.op("act", lambda e: e.activation(out=owb[:], in_=powin[:, 0:260].rearrange("p (r c) -> p r c", c=65), func=AF.Copy), reads=[powin.b], writes=[owb.b])
                P.op("dve", lambda e: e.reciprocal(out=rsel[:], in_=osb[:, :, 64]), reads=[osb.b], writes=[rsel.b])
                P.op("dve", lambda e: e.reciprocal(out=rwin[:], in_=owb[:, :, 64]), reads=[owb.b], writes=[rwin.b])
                P.op("dve", lambda e, gv=gv: e.tensor_mul(out=wsel[:], in0=rsel[:], in1=gv[:, :, 1]), reads=[rsel.b, gts[j].b], writes=[wsel.b])
                P.op("dve", lambda e, gv=gv: e.tensor_mul(out=wwin[:], in0=rwin[:], in1=gv[:, :, 2]), reads=[rwin.b, gts[j].b], writes=[wwin.b])
                o = ot[j]
                P.op("dve", lambda e, o=o, gv=gv: e.tensor_mul(out=o[:], in0=poc[:], in1=gv[:, :, 0:1].to_broadcast([128, 4, 64])), reads=[poc.b, gts[j].b], writes=[o.b])
                P.op("dve", lambda e: e.tensor_mul(out=t2[:], in0=osb[:, :, 0:64], in1=wsel[:].unsqueeze(2).to_broadcast([128, 4, 64])), reads=[osb.b, wsel.b], writes=[t2.b])
                P.op("dve", lambda e, o=o: e.tensor_add(out=o[:], in0=o[:], in1=t2[:]), reads=[o.b, t2.b], writes=[o.b])
                P.op("dve", lambda e: e.tensor_mul(out=t2[:], in0=owb[:, :, 0:64], in1=wwin[:].unsqueeze(2).to_broadcast([128, 4, 64])), reads=[owb.b, wwin.b], writes=[t2.b])
                P.op("dve", lambda e, o=o: e.tensor_add(out=o[:], in0=o[:], in1=t2[:]), reads=[o.b, t2.b], writes=[o.b])
                P.dma("pool", lambda e, o=o, r0=r0, g=g: e.dma_start(out=Sx["ONSA"][r0:r0 + 128, g * 256:(g + 1) * 256], in_=o[:].rearrange("p r d -> p (r d)")),
                      P.S(("o", j)), reads=[o.b], writes=[db("ONSA", ti, g)])
    P.end_phase()
    es.close()


def phase_c(C):
    nc, P, I, Sx, db = C.nc, C.P, C.I, C.Sx, C.db
    NSEQ, S, NTS = C.NSEQ, C.S, C.NTS
    es = ExitStack()
    sb, ps = mk_tiles(C, es)
    ident = sb("c_ident", [128, 128], BF16)
    identf = sb("c_identf", [128, 128])
    cw = sb("c_cw", [128, 8, 4])
    cb = sb("c_cb", [128, 8])
    dtb = sb("c_dtb", [128, 8])
    aneg = sb("c_aneg", [128, 8])
    dsk = sb("c_dsk", [128, 8])
    sng = sb("c_sng", [128, 512])
    tri = sb("c_tri", [128, 128])
    onesf = sb("c_ones", [128, 128])
    mneg = sb("c_mneg", [128, 128])
    junk = sb("c_junk", [128, 512])
    NB = 2
    xin = [sb(f"c_xin{j}", [128, 8, 131]) for j in range(NB)]
    dtt = [sb(f"c_dt{j}", [128, 8]) for j in range(NB)]
    zt = [sb(f"c_z{j}", [128, 512]) for j in range(NB)]
    acc = sb("c_acc", [128, 8, 128])
    xc = sb("c_xc", [128, 8, 128])
    bcT = sb("c_bcT", [128, 4, 128], BF16)
    bmtm = sb("c_bmtm", [128, 2, 128], BF16)
    xstm = sb("c_xstm", [128, 512])
    dte = sb("c_dte", [128, 8])
    dts = sb("c_dts", [128, 8])
    adt = sb("c_adt", [128, 8])
    acol = sb("c_acol", [128, 8])
    ea = sb("c_ea", [128, 8])
    Rr = sb("c_R", [128, 8, 128])
    alast = sb("c_alast", [128, 8])
    dsc = sb("c_dsc", [128, 8])
    cdec = sb("c_cdec", [128, 8])
    Dm = [sb(f"c_Dm{j}", [128, 128]) for j in range(2)]
    seg = [sb(f"c_seg{j}", [128, 128]) for j in range(2)]
    MT = [sb(f"c_MT{j}", [128, 128], BF16) for j in range(2)]
    xdt = sb("c_xdt", [128, 8, 64], BF16)
    xdtd = sb("c_xdtd", [128, 8, 64], BF16)
    Hst = sb("c_H", [128, 8, 64])
    Hb = sb("c_Hb", [128, 8, 64], BF16)
    yd = sb("c_yd", [128, 8, 64])
    y = sb("c_y", [128, 8, 64])
    y2 = sb("c_y2", [128, 8, 64])
    ssq = sb("c_ssq", [128, 2])
    rstd = sb("c_rstd", [128, 2])
    yo = [sb(f"c_yo{j}", [128, 512]) for j in range(NB)]
    pXT = ps("c_pXT", [128, 512])
    pBT = ps("c_pBT", [128, 512])
    pABC = [ps(f"c_pABC{j}", [128, 4, 128]) for j in range(2)]
    pCB = ps("c_pCB", [128, 2, 128])
    pYD = ps("c_pYD", [128, 8, 64])
    pYO = ps("c_pYO", [128, 8, 64])
    pST = ps("c_pST", [128, 8, 64])

    make_ident(C, ident, identf)
    for k in range(4):
        P.dma("sp", lambda e, k=k: e.dma_start(out=cw[:, :, k], in_=I["conv_w"][k, :].rearrange("(c p) -> p c", p=128), allow_slow_non_contiguous=True),
              P.S("cw"), writes=[cw.b])
    P.dma("sp", lambda e: e.dma_start(out=cb[:], in_=I["conv_b"].rearrange("(c p) o -> p (c o)", p=128), allow_slow_non_contiguous=True), P.S("cb"), writes=[cb.b])
    P.dma("sp", lambda e: e.dma_start(out=dtb[:], in_=I["dt_bias"].partition_broadcast(128)), P.S("dtb"), writes=[dtb.b])
    P.dma("sp", lambda e: e.dma_start(out=aneg[:], in_=I["a_log"].partition_broadcast(128)), P.S("aneg"), writes=[aneg.b])
    P.dma("sp", lambda e: e.dma_start(out=dsk[:], in_=I["d_skip"].partition_broadcast(128)), P.S("dsk"), writes=[dsk.b])
    P.dma("sp", lambda e: e.dma_start(out=sng[:], in_=I["ssd_norm_g"].partition_broadcast(128)), P.S("sng"), writes=[sng.b])
    P.op("act", lambda e: e.activation(out=aneg[:], in_=aneg[:], func=AF.Exp), reads=[aneg.b], writes=[aneg.b])
    P.op("dve", lambda e: e.tensor_scalar_mul(out=aneg[:], in0=aneg[:], scalar1=-1.0), reads=[aneg.b], writes=[aneg.b])
    P.op("pool", lambda e: e.memset(tri[:], 1.0), writes=[tri.b])
    P.op("pool", lambda e: e.affine_select(out=tri[:], in_=tri[:], pattern=[[1, 128]], compare_op=ALU.is_ge, fill=0.0, base=0, channel_multiplier=-1),
         reads=[tri.b], writes=[tri.b])
    P.op("pool", lambda e: e.memset(mneg[:], 0.0), writes=[mneg.b])
    P.op("pool", lambda e: e.affine_select(out=mneg[:], in_=mneg[:], pattern=[[1, 128]], compare_op=ALU.is_ge, fill=NEG, base=0, channel_multiplier=-1),
         reads=[mneg.b], writes=[mneg.b])
    P.op("pool", lambda e: e.memset(onesf[:], 1.0), writes=[onesf.b])

    cnt = 0
    for seq in range(NSEQ):
        P.op("dve", lambda e: e.memset(Hst[:], 0.0), reads=[Hst.b], writes=[Hst.b])
        P.op("dve", lambda e: e.memset(Hb[:], 0.0), reads=[Hb.b], writes=[Hb.b])
        for c in range(NTS):
            j = cnt % NB
            cnt += 1
            ti = seq * NTS + c
            r0 = ti * 128
            t0 = c * 128
            X = xin[j]
            xsrc = Sx["XBC"][seq].rearrange("(c p) t -> p c t", p=128)
            if c == 0:
                P.op("dve", lambda e, X=X: e.memset(X[:, :, 0:3], 0.0), reads=[X.b], writes=[X.b])
                P.dma("sp", lambda e, X=X, xsrc=xsrc: e.dma_start(out=X[:, :, 3:131], in_=xsrc[:, :, 0:128]), P.S(("x", j)), reads=[db("XBC", seq, 0)], writes=[X.b])
            else:
                P.dma("sp", lambda e, X=X, xsrc=xsrc, t0=t0: e.dma_start(out=X[:, :, 0:131], in_=xsrc[:, :, t0 - 3:t0 + 128]), P.S(("x", j)),
                      reads=[db("XBC", seq, c), db("XBC", seq, c - 1)], writes=[X.b])
            P.dma("sp", lambda e, j=j, r0=r0: e.dma_start(out=dtt[j][:], in_=Sx["DT"][r0:r0 + 128, :]), P.S(("dt", j)), reads=[db("DT", ti)], writes=[dtt[j].b])
            P.dma("sp", lambda e, j=j, r0=r0: e.dma_start(out=zt[j][:], in_=Sx["Z"][r0:r0 + 128, :]), P.S(("z", j)), reads=[db("Z", ti)], writes=[zt[j].b])
            for cc in range(8):
                P.op("dve", lambda e, X=X, cc=cc: e.tensor_scalar(out=acc[:, cc, :], in0=X[:, cc, 0:128], scalar1=cw[:, cc, 0:1], scalar2=cb[:, cc:cc + 1],
                                                                  op0=ALU.mult, op1=ALU.add), reads=[X.b, cw.b, cb.b, acc.b], writes=[acc.b])
                for k in range(1, 4):
                    P.op("dve", lambda e, X=X, cc=cc, k=k: e.scalar_tensor_tensor(out=acc[:, cc, :], in0=X[:, cc, k:k + 128], scalar=cw[:, cc, k:k + 1], in1=acc[:, cc, :],
                                                                                  op0=ALU.mult, op1=ALU.add), reads=[X.b, cw.b, acc.b], writes=[acc.b])
            P.op("act", lambda e: e.activation(out=xc[:], in_=acc[:], func=AF.Silu), reads=[acc.b], writes=[xc.b])
            P.op("dve", lambda e: e.tensor_copy(out=bcT[:], in_=xc[:, 4:8, :]), reads=[xc.b], writes=[bcT.b])
            for cc in range(4):
                P.op("pe", lambda e, cc=cc: e.transpose(out=pXT[:, cc * 128:(cc + 1) * 128], in_=xc[:, cc, :], identity=identf[:]), reads=[xc.b, identf.b], writes=[pXT.b])
            P.op("act", lambda e: e.activation(out=xstm[:], in_=pXT[:], func=AF.Copy), reads=[pXT.b], writes=[xstm.b])
            for g in range(2):
                P.op("pe", lambda e, g=g: e.transpose(out=pBT[:, g * 128:(g + 1) * 128], in_=xc[:, 4 + g, :], identity=identf[:]), reads=[xc.b, identf.b], writes=[pBT.b])
            P.op("act", lambda e: e.activation(out=bmtm[:], in_=pBT[:, 0:256].rearrange("p (g n) -> p g n", g=2), func=AF.Copy), reads=[pBT.b], writes=[bmtm.b])
            P.op("dve", lambda e, j=j: e.tensor_add(out=dte[:], in0=dtt[j][:], in1=dtb[:]), reads=[dtt[j].b, dtb.b], writes=[dte.b])
            P.op("act", lambda e: e.activation(out=dte[:], in_=dte[:], func=AF.Exp), reads=[dte.b], writes=[dte.b])
            P.op("act", lambda e: e.activation(out=dts[:], in_=dte[:], func=AF.Ln, bias=1.0), reads=[dte.b], writes=[dts.b])
            P.op("dve", lambda e: e.tensor_mul(out=adt[:], in0=dts[:], in1=aneg[:]), reads=[dts.b, aneg.b], writes=[adt.b])
            P.op("pe", lambda e: e.matmul(pBT[:, 256:264], lhsT=tri[:], rhs=adt[:], start=True, stop=True), reads=[tri.b, adt.b], writes=[pBT.b])
            P.op("dve", lambda e: e.tensor_copy(out=acol[:], in_=pBT[:, 256:264]), reads=[pBT.b], writes=[acol.b])
            P.op("act", lambda e: e.activation(out=ea[:], in_=pBT[:, 256:264], func=AF.Exp), reads=[pBT.b], writes=[ea.b])
            for h in range(8):
                P.op("dve", lambda e, h=h: e.tensor_scalar_mul(out=Rr[:, h, :], in0=tri[:], scalar1=adt[:, h:h + 1]), reads=[tri.b, adt.b, Rr.b], writes=[Rr.b])
            for hh in range(2):
                P.op("pe", lambda e, hh=hh: e.matmul(pABC[hh][:].rearrange("p h l -> p (h l)"), lhsT=onesf[:], rhs=Rr[:, hh * 4:(hh + 1) * 4, :].rearrange("p h l -> p (h l)"),
                                                    start=True, stop=True), reads=[onesf.b, Rr.b], writes=[pABC[hh].b])
            for hh in range(2):
                P.op("dve", lambda e, hh=hh: e.tensor_copy(out=alast[:, hh * 4:(hh + 1) * 4], in_=pABC[hh][:, :, 127]), reads=[pABC[hh].b, alast.b], writes=[alast.b])
            P.op("dve", lambda e: e.tensor_sub(out=dsc[:], in0=alast[:], in1=acol[:]), reads=[alast.b, acol.b], writes=[dsc.b])
            P.op("act", lambda e: e.activation(out=dsc[:], in_=dsc[:], func=AF.Exp), reads=[dsc.b], writes=[dsc.b])
            P.op("act", lambda e: e.activation(out=cdec[:], in_=alast[:], func=AF.Exp), reads=[alast.b], writes=[cdec.b])
            xs3 = xstm[:].rearrange("p (h d) -> p h d", d=64)
            P.op("dve", lambda e, xs3=xs3: e.tensor_mul(out=xdt[:], in0=xs3, in1=dts[:].unsqueeze(2).to_broadcast([128, 8, 64])), reads=[xstm.b, dts.b], writes=[xdt.b])
            P.op("dve", lambda e: e.tensor_mul(out=xdtd[:], in0=xdt[:], in1=dsc[:].unsqueeze(2).to_broadcast([128, 8, 64])), reads=[xdt.b, dsc.b], writes=[xdtd.b])
            for g in range(2):
                P.op("pe", lambda e, g=g: e.matmul(pCB[:, g, :], lhsT=bcT[:, g, :], rhs=bcT[:, 2 + g, :], start=True, stop=True), reads=[bcT.b], writes=[pCB.b])
            for h in range(8):
                g = h // 4
                k2 = h % 2
                P.op("dve", lambda e, h=h, k2=k2: e.scalar_tensor_tensor(out=Dm[k2][:], in0=pABC[h // 4][:, h % 4, :], scalar=acol[:, h:h + 1], in1=mneg[:],
                                                                         op0=ALU.subtract, op1=ALU.add), reads=[pABC[h // 4].b, acol.b, mneg.b], writes=[Dm[k2].b])
                P.op("act", lambda e, k2=k2: e.activation(out=seg[k2][:], in_=Dm[k2][:], func=AF.Exp), reads=[Dm[k2].b], writes=[seg[k2].b])
                P.op("dve", lambda e, g=g, k2=k2: e.tensor_mul(out=MT[k2][:], in0=pCB[:, g, :], in1=seg[k2][:]), reads=[pCB.b, seg[k2].b], writes=[MT[k2].b])
                P.op("pe", lambda e, h=h, k2=k2: e.matmul(pYD[:, h, :], lhsT=MT[k2][:], rhs=xdt[:, h, :], start=True, stop=True), reads=[MT[k2].b, xdt.b], writes=[pYD.b])
                P.op("pe", lambda e, h=h, g=g: e.matmul(pYO[:, h, :], lhsT=bcT[:, 2 + g, :], rhs=Hb[:, h, :], start=True, stop=True), reads=[bcT.b, Hb.b], writes=[pYO.b])
                P.op("pe", lambda e, h=h, g=g: e.matmul(pST[:, h, :], lhsT=bmtm[:, g, :], rhs=xdtd[:, h, :], start=True, stop=True), reads=[bmtm.b, xdtd.b], writes=[pST.b])
            P.op("act", lambda e: e.activation(out=yd[:], in_=pYD[:], func=AF.Copy), reads=[pYD.b], writes=[yd.b])
            P.op("dve", lambda e: e.tensor_mul(out=y[:], in0=pYO[:], in1=ea[:].unsqueeze(2).to_broadcast([128, 8, 64])), reads=[pYO.b, ea.b], writes=[y.b])
            P.op("dve", lambda e: e.tensor_add(out=y[:], in0=y[:], in1=yd[:]), reads=[y.b, yd.b], writes=[y.b])
            P.op("dve", lambda e, xs3=xs3: e.tensor_mul(out=y2[:], in0=xs3, in1=dsk[:].unsqueeze(2).to_broadcast([128, 8, 64])), reads=[xstm.b, dsk.b], writes=[y2.b])
            P.op("dve", lambda e: e.tensor_add(out=y[:], in0=y[:], in1=y2[:]), reads=[y.b, y2.b], writes=[y.b])
            yf = y[:].rearrange("p h d -> p (h d)")
            P.op("dve", lambda e, yf=yf, j=j: e.tensor_mul(out=yf, in0=yf, in1=zt[j][:]), reads=[y.b, zt[j].b], writes=[y.b])
            for g in range(2):
                P.op("act", lambda e, g=g, yf=yf: e.activation(out=junk[:, 0:256], in_=yf[:, g * 256:(g + 1) * 256], func=AF.Square, accum_out=ssq[:, g:g + 1]),
                     reads=[y.b, ssq.b], writes=[ssq.b])
            P.op("act", lambda e: e.activation(out=rstd[:], in_=ssq[:], func=AF.Ln, scale=1.0 / 256, bias=EPS), reads=[ssq.b], writes=[rstd.b])
            P.op("act", lambda e: e.activation(out=rstd[:], in_=rstd[:], func=AF.Exp, scale=-0.5), reads=[rstd.b], writes=[rstd.b])
            for g in range(2):
                P.op("dve", lambda e, g=g, j=j, yf=yf: e.scalar_tensor_tensor(out=yo[j][:, g * 256:(g + 1) * 256], in0=yf[:, g * 256:(g + 1) * 256], scalar=rstd[:, g:g + 1],
                                                                             in1=sng[:, g * 256:(g + 1) * 256], op0=ALU.mult, op1=ALU.mult),
                     reads=[y.b, rstd.b, sng.b, yo[j].b], writes=[yo[j].b])
            P.dma("pool", lambda e, j=j, r0=r0: e.dma_start(out=Sx["MIX"][r0:r0 + 128, 512:1024], in_=yo[j][:]), P.S(("o", j)), reads=[yo[j].b], writes=[db("MIXS", ti)])
            P.op("dve", lambda e: e.tensor_mul(out=Hst[:], in0=Hst[:], in1=cdec[:].unsqueeze(2).to_broadcast([128, 8, 64])), reads=[Hst.b, cdec.b], writes=[Hst.b])
            P.op("dve", lambda e: e.tensor_add(out=Hst[:], in0=Hst[:], in1=pST[:]), reads=[Hst.b, pST.b], writes=[Hst.b])
            P.op("act", lambda e: e.activation(out=Hb[:], in_=Hst[:], func=AF.Copy), reads=[Hst.b], writes=[Hb.b])
    P.end_phase()
    es.close()


def phase_d(C):
    nc, P, I, Sx, db = C.nc, C.P, C.I, C.Sx, C.db
    NSEQ, S, NTS, T = C.NSEQ, C.S, C.NTS, C.T
    es = ExitStack()
    sb, ps = mk_tiles(C, es)
    ident = sb("d_ident", [128, 128], BF16)
    identf = sb("d_identf", [128, 128])
    Wout = sb("d_Wout", [128, 8, 1024], BF16)
    Wq = sb("d_Wq", [128, 8, 2048], BF16)
    keysT = sb("d_keysT", [128, 16, 128], BF16)
    nsag = sb("d_nsag", [128, 512])
    ffng = sb("d_ffng", [128, 1024])
    fing = sb("d_fing", [128, 1024])
    onsa = sb("d_onsa", [128, 512])
    mixs = sb("d_mixs", [128, 512])
    mixb = sb("d_mixb", [128, 1024], BF16)
    mixT = sb("d_mixT", [128, 8, 128], BF16)
    xt = sb("d_xt", [128, 1024])
    acc = sb("d_acc", [128, 1024])
    hn = sb("d_hn", [128, 1024])
    hnB = sb("d_hnB", [128, 1024])
    hnbs = [sb(f"d_hnb{j}", [128, 1024], BF16) for j in range(2)]
    prodb = [sb(f"d_prodb{j}", [128, 1024], BF16) for j in range(2)]
    hnT = mixT
    qT = sb("d_qT", [128, 16, 128], BF16)
    sc = sb("d_sc", [128, 16, 128])
    sc2 = sb("d_sc2", [128, 16, 128])
    m16 = sb("d_m16", [128, 16, 16])
    i16u = sb("d_i16u", [128, 16, 16], U32)
    i16f = sb("d_i16f", [128, 16, 16])
    cand = View(sc[:].rearrange("p (h x) k -> p h (x k)", h=8), sc.b)
    abu = sb("d_abu", [128, 2, 128], U32)
    abf = sb("d_abf", [128, 2, 128])
    e12 = sb("d_e12", [128, 2, 128])
    ug = sb("d_ug", [128, 128])
    vals = sb("d_vals", [128, 8, 16])
    posu = sb("d_posu", [128, 8, 16], U32)
    iota_i = sb("d_iota_i", [128, 256], I32)
    iota_f = sb("d_iota_f", [128, 256])
    eidf = sb("d_eidf", [128, 128])
    eidi = sb("d_eidi", [128, 128], I32)
    junk = sb("d_junk", [128, 1024])
    u = sb("d_u", [128, 128])
    g1 = sb("d_g1", [128, 128])
    g2 = sb("d_g2", [128, 128])
    gate = sb("d_gate", [128, 8, 16])
    gsum = sb("d_gsum", [128, 8])
    gh = sb("d_gh", [128, 128])
    epsb = sb("d_epsb", [128, 1])
    ssq = sb("d_ssq", [128, 1])
    rstd = sb("d_rstd", [128, 1])
    NG = 16
    GS = 4
    Guv = [sb(f"d_Guv{j}", [128, 2048], BF16) for j in range(NG)]

    ot = xt
    pT = ps("d_pT", [128, 8, 128], BF16)
    pO = [ps(f"d_pO{j}", [128, 512]) for j in range(2)]
    pQ = [ps(f"d_pQ{j}", [128, 4, 128]) for j in range(2)]
    pV = [ps(f"d_pV{j}", [128, 512]) for j in range(2)]
    diags = [sb(f"d_diag{j}", [128, 128], BF16) for j in range(8)]

    make_ident(C, ident, identf)
    UV, tab = C.UV, C.tab
    P.op("dve", lambda e: e.memset(epsb[:], EPS), writes=[epsb.b])
    P.op("pool", lambda e: e.iota(iota_i[:], pattern=[[1, 256]], base=0, channel_multiplier=0), writes=[iota_i.b])
    P.op("dve", lambda e: e.tensor_copy(out=iota_f[:], in_=iota_i[:]), reads=[iota_i.b], writes=[iota_f.b])
    wo = I["w_out"].rearrange("(c p) n -> p c n", p=128)
    wq = I["peer_w_q"].rearrange("(c p) n -> p c n", p=128)
    for c in range(8):
        P.dma("pool", lambda e, c=c: e.dma_start(out=Wout[:, c, :], in_=wo[:, c, :]), P.S("wout"), writes=[Wout.b])
        P.dma("pool", lambda e, c=c: e.dma_start(out=Wq[:, c, :], in_=wq[:, c, :]), P.S("wq"), writes=[Wq.b])
    P.dma("sp", lambda e: e.dma_start(out=nsag[:], in_=I["nsa_norm_g"].partition_broadcast(128)), P.S("nsag"), writes=[nsag.b])
    P.dma("sp", lambda e: e.dma_start(out=ffng[:], in_=I["ffn_norm_g"].partition_broadcast(128)), P.S("ffng"), writes=[ffng.b])
    P.dma("sp", lambda e: e.dma_start(out=fing[:], in_=I["final_norm_g"].partition_broadcast(128)), P.S("fing"), writes=[fing.b])
    P.dma("sp", lambda e: e.dma_start(out=sc[:], in_=I["peer_keys"].rearrange("a k d -> k a d")), P.S("keys"), writes=[sc.b])
    for b4 in range(4):
        for a in range(b4 * 4, b4 * 4 + 4):
            P.op("pe", lambda e, a=a, b4=b4: e.transpose(out=pQ[b4 % 2][:, a % 4, :], in_=sc[:, a, :], identity=identf[:]), reads=[sc.b, identf.b], writes=[pQ[b4 % 2].b])
        P.op("act", lambda e, b4=b4: e.activation(out=keysT[:, b4 * 4:(b4 + 1) * 4, :], in_=pQ[b4 % 2][:], func=AF.Copy), reads=[pQ[b4 % 2].b, keysT.b], writes=[keysT.b])

    def rms(src_ap, srcb, n):
        P.op("act", lambda e: e.activation(out=junk[:, 0:n], in_=src_ap, func=AF.Square, accum_out=ssq[:]), reads=[srcb], writes=[ssq.b])
        P.op("act", lambda e: e.activation(out=rstd[:], in_=ssq[:], func=AF.Ln, scale=1.0 / n, bias=epsb[:]), reads=[ssq.b, epsb.b], writes=[rstd.b])
        P.op("act", lambda e: e.activation(out=rstd[:], in_=rstd[:], func=AF.Exp, scale=-0.5), reads=[rstd.b], writes=[rstd.b])

    accs = [acc, sb('d_accB', [128, 1024])]
    eidis = [eidi, sb('d_eidiB', [128, 128], I32)]

    hns = [hn, hnB]

    def stage1(ti):
        acc, eidi, hn, hnb = accs[ti % 2], eidis[ti % 2], hns[ti % 2], hnbs[ti % 2]
        r0 = ti * 128
        P.dma("sp", lambda e, r0=r0: e.dma_start(out=onsa[:], in_=Sx["ONSA"][r0:r0 + 128, :]), P.S("onsa"), reads=[db("ONSA", ti, 0), db("ONSA", ti, 1)], writes=[onsa.b])
        P.dma("sp", lambda e, r0=r0: e.dma_start(out=mixs[:], in_=Sx["MIX"][r0:r0 + 128, 512:1024]), P.S("mixs"), reads=[db("MIXS", ti)], writes=[mixs.b])
        P.dma("sp", lambda e, r0=r0: e.dma_start(out=xt[:], in_=I["x"][r0:r0 + 128, :]), P.S("xt"), writes=[xt.b])
        rms(onsa[:], onsa.b, 512)
        P.op("dve", lambda e: e.scalar_tensor_tensor(out=mixb[:, 0:512], in0=onsa[:], scalar=rstd[:, 0:1], in1=nsag[:], op0=ALU.mult, op1=ALU.mult),
             reads=[onsa.b, rstd.b, nsag.b, mixb.b], writes=[mixb.b])
        P.op("act", lambda e: e.activation(out=mixb[:, 512:1024], in_=mixs[:], func=AF.Copy), reads=[mixs.b, mixb.b], writes=[mixb.b])
        for c in range(8):
            P.op("pe", lambda e, c=c: e.transpose(out=pT[:, c, :], in_=mixb[:, c * 128:(c + 1) * 128], identity=ident[:]), reads=[mixb.b, ident.b], writes=[pT.b])
        P.op("act", lambda e: e.activation(out=mixT[:], in_=pT[:], func=AF.Copy), reads=[pT.b], writes=[mixT.b])
        for n2 in range(2):
            for c in range(8):
                P.op("pe", lambda e, n2=n2, c=c: e.matmul(pO[n2][:], lhsT=mixT[:, c, :], rhs=Wout[:, c, n2 * 512:(n2 + 1) * 512], start=(c == 0), stop=(c == 7)),
                     reads=[mixT.b, Wout.b], writes=[pO[n2].b])
        for n2 in range(2):
            P.op("dve", lambda e, n2=n2: e.tensor_add(out=acc[:, n2 * 512:(n2 + 1) * 512], in0=pO[n2][:], in1=xt[:, n2 * 512:(n2 + 1) * 512]),
                 reads=[pO[n2].b, xt.b, acc.b], writes=[acc.b])
        rms(acc[:], acc.b, 1024)
        P.op("dve", lambda e: e.scalar_tensor_tensor(out=hn[:], in0=acc[:], scalar=rstd[:, 0:1], in1=ffng[:], op0=ALU.mult, op1=ALU.mult),
             reads=[acc.b, rstd.b, ffng.b], writes=[hn.b])
        P.op("act", lambda e: e.activation(out=hnb[:], in_=hn[:], func=AF.Copy), reads=[hn.b], writes=[hnb.b])
        for c in range(8):
            P.op("pe", lambda e, c=c: e.transpose(out=pT[:, c, :], in_=hnb[:, c * 128:(c + 1) * 128], identity=ident[:]), reads=[hnb.b, ident.b], writes=[pT.b])
        P.op("act", lambda e: e.activation(out=hnT[:], in_=pT[:], func=AF.Copy), reads=[pT.b], writes=[hnT.b])
        for b4 in range(4):
            pq = pQ[b4 % 2]
            for hc in range(b4 * 4, b4 * 4 + 4):
                for c in range(8):
                    P.op("pe", lambda e, hc=hc, c=c, pq=pq: e.matmul(pq[:, hc % 4, :], lhsT=Wq[:, c, hc * 128:(hc + 1) * 128], rhs=hnT[:, c, :], start=(c == 0), stop=(c == 7)),
                         reads=[Wq.b, hnT.b], writes=[pq.b])
            P.op("act", lambda e, b4=b4, pq=pq: e.activation(out=qT[:, b4 * 4:(b4 + 1) * 4, :], in_=pq[:], func=AF.Copy), reads=[pq.b, qT.b], writes=[qT.b])
        for grp in range(4):
            pb = pO[grp % 2]
            for k4 in range(4):
                hc = grp * 4 + k4
                P.op("pe", lambda e, pb=pb, k4=k4, hc=hc: e.matmul(pb[:, k4 * 128:(k4 + 1) * 128], lhsT=qT[:, hc, :], rhs=keysT[:, hc, :], start=True, stop=True),
                     reads=[qT.b, keysT.b], writes=[pb.b])
            P.op("act", lambda e, pb=pb, grp=grp: e.activation(out=sc[:, grp * 4:(grp + 1) * 4, :], in_=pb[:].rearrange("p (a k) -> p a k", a=4), func=AF.Copy),
                 reads=[pb.b, sc.b], writes=[sc.b])
        for hc in range(16):
            P.op("dve", lambda e, hc=hc: e.max(out=m16[:, hc, 0:8], in_=sc[:, hc, :]), reads=[sc.b, m16.b], writes=[m16.b])
            P.op("dve", lambda e, hc=hc: e.max_index(out=i16u[:, hc, 0:8], in_max=m16[:, hc, 0:8], in_values=sc[:, hc, :]), reads=[sc.b, m16.b, i16u.b], writes=[i16u.b])
            P.op("dve", lambda e, hc=hc: e.match_replace(out=sc2[:, hc, :], in_to_replace=m16[:, hc, 0:8], in_values=sc[:, hc, :], imm_value=-1e30),
                 reads=[sc.b, m16.b, sc2.b], writes=[sc2.b])
            P.op("dve", lambda e, hc=hc: e.max(out=m16[:, hc, 8:16], in_=sc2[:, hc, :]), reads=[sc2.b, m16.b], writes=[m16.b])
            P.op("dve", lambda e, hc=hc: e.max_index(out=i16u[:, hc, 8:16], in_max=m16[:, hc, 8:16], in_values=sc2[:, hc, :]), reads=[sc2.b, m16.b, i16u.b], writes=[i16u.b])
        P.op("dve", lambda e: e.tensor_copy(out=i16f[:], in_=i16u[:]), reads=[i16u.b], writes=[i16f.b])
        c4 = cand[:].rearrange("p h (a b) -> p h a b", a=16)
        P.op("dve", lambda e, c4=c4: e.tensor_tensor(out=c4, in0=m16[:, 0::2, :].unsqueeze(3).to_broadcast([128, 8, 16, 16]),
                                                     in1=m16[:, 1::2, :].unsqueeze(2).to_broadcast([128, 8, 16, 16]), op=ALU.add), reads=[m16.b], writes=[cand.b])
        cand2 = sc2[:].rearrange("p (h x) k -> p h (x k)", h=8)
        for h in range(8):
            P.op("dve", lambda e, h=h: e.max(out=vals[:, h, 0:8], in_=cand[:, h, :]), reads=[cand.b, vals.b], writes=[vals.b])
            P.op("dve", lambda e, h=h: e.max_index(out=posu[:, h, 0:8], in_max=vals[:, h, 0:8], in_values=cand[:, h, :]), reads=[cand.b, vals.b, posu.b], writes=[posu.b])
            P.op("dve", lambda e, h=h, cand2=cand2: e.match_replace(out=cand2[:, h, :], in_to_replace=vals[:, h, 0:8], in_values=cand[:, h, :], imm_value=-1e30),
                 reads=[cand.b, vals.b, sc2.b], writes=[sc2.b])
            P.op("dve", lambda e, h=h, cand2=cand2: e.max(out=vals[:, h, 8:16], in_=cand2[:, h, :]), reads=[sc2.b, vals.b], writes=[vals.b])
            P.op("dve", lambda e, h=h, cand2=cand2: e.max_index(out=posu[:, h, 8:16], in_max=vals[:, h, 8:16], in_values=cand2[:, h, :]), reads=[sc2.b, vals.b, posu.b], writes=[posu.b])
        pflat = posu[:].rearrange("p h k -> p (h k)")
        P.op("dve", lambda e, pflat=pflat: e.tensor_single_scalar(out=abu[:, 0, :], in_=pflat, scalar=4, op=ALU.logical_shift_right), reads=[posu.b, abu.b], writes=[abu.b])
        P.op("dve", lambda e, pflat=pflat: e.tensor_single_scalar(out=abu[:, 1, :], in_=pflat, scalar=15, op=ALU.bitwise_and), reads=[posu.b, abu.b], writes=[abu.b])
        P.op("dve", lambda e: e.tensor_copy(out=abf[:], in_=abu[:]), reads=[abu.b], writes=[abf.b])
        T4 = sc2[:].rearrange("p a k -> p (a k)").rearrange("p (h k a) -> p h k a", h=8, k=16)
        io16 = iota_f[:, 0:16].unsqueeze(1).unsqueeze(1).to_broadcast([128, 8, 16, 16])
        for w in range(2):
            sel = abf[:, w, :].rearrange("p (h k) -> p h k", h=8).unsqueeze(3).to_broadcast([128, 8, 16, 16])
            tabv = i16f[:, w::2, :].unsqueeze(2).to_broadcast([128, 8, 16, 16])
            P.op("dve", lambda e, sel=sel, T4=T4, io16=io16: e.tensor_tensor(out=T4, in0=sel, in1=io16, op=ALU.is_equal), reads=[abf.b, iota_f.b, sc2.b], writes=[sc2.b])
            P.op("dve", lambda e, T4=T4, tabv=tabv: e.tensor_tensor(out=T4, in0=T4, in1=tabv, op=ALU.mult), reads=[sc2.b, i16f.b], writes=[sc2.b])
            P.op("dve", lambda e, T4=T4, w=w: e.tensor_reduce(out=e12[:, w, :].rearrange("p (h k) -> p h k", h=8), in_=T4, axis=AX.X, op=ALU.add),
                 reads=[sc2.b, e12.b], writes=[e12.b])
        P.op("dve", lambda e: e.scalar_tensor_tensor(out=eidf[:], in0=e12[:, 0, :], scalar=128.0, in1=e12[:, 1, :], op0=ALU.mult, op1=ALU.add),
             reads=[e12.b], writes=[eidf.b])
        P.op("dve", lambda e: e.tensor_copy(out=eidi[:], in_=eidf[:]), reads=[eidf.b], writes=[eidi.b])


    junkp = sb("d_junkp", [128, 1024])
    ub = [Buf(f"u{g}") for g in range(128 // GS)]
    ub2 = [Buf(f"u2{g}") for g in range(128 // GS)]
    g1b = [Buf(f"g1{g}") for g in range(128 // GS)]
    ghb = [Buf(f"gh{g}") for g in range(128 // GS)]

    def gphase(ti, thunks):
        acc, eidi, hn, hnb = accs[ti % 2], eidis[ti % 2], hns[ti % 2], hnbs[ti % 2]
        r0 = ti * 128
        NGRP = 128 // GS
        per = (len(thunks) + 127) // 128
        P.op("dve", lambda e: e.tensor_sub(out=gate[:], in0=vals[:], in1=vals[:, :, 0:1].to_broadcast([128, 8, 16])), reads=[vals.b], writes=[gate.b])
        P.op("act", lambda e: e.activation(out=gate[:], in_=gate[:], func=AF.Exp), reads=[gate.b], writes=[gate.b])
        P.op("dve", lambda e: e.tensor_reduce(out=gsum[:], in_=gate[:], axis=AX.X, op=ALU.add), reads=[gate.b], writes=[gsum.b])
        P.op("dve", lambda e: e.reciprocal(out=gsum[:], in_=gsum[:]), reads=[gsum.b], writes=[gsum.b])
        P.op("dve", lambda e: e.tensor_mul(out=gate[:], in0=gate[:], in1=gsum[:].unsqueeze(2).to_broadcast([128, 8, 16])), reads=[gate.b, gsum.b], writes=[gate.b])
        gflat = gate[:].rearrange("p h k -> p (h k)")
        for step in range(NGRP + 2):
            if step >= 2:
                g = step - 2
                cs = slice(g * GS, (g + 1) * GS)
                P.op("dve", lambda e, cs=cs: e.tensor_mul(out=gh[:, cs], in0=g2[:, cs], in1=ug[:, cs]), reads=[g1b[g], ghb[g]], writes=[ghb[g]])
                for s_ in range(GS):
                    jx = g * GS + s_
                    G = Guv[jx % NG]
                    dg = diags[jx % 8]
                    P.op("act", lambda e, jx=jx, dg=dg: e.activation(out=dg[:], in_=ident[:], func=AF.Copy, scale=gh[:, jx:jx + 1]), reads=[ident.b, ghb[g]], writes=[dg.b])
                    for n2 in range(2):
                        P.op("pe", lambda e, dg=dg, G=G, n2=n2, jx=jx: e.matmul(pV[n2][:], lhsT=dg[:], rhs=G[:, 1024 + n2 * 512:1024 + (n2 + 1) * 512], start=(jx == 0), stop=(jx == 127)),
                             reads=[dg.b, G.b], writes=[pV[n2].b])
            if step < NGRP:
                for s_ in range(GS):
                    jx = step * GS + s_
                    G = Guv[jx % NG]
                    P.dma("pool", lambda e, G=G, jx=jx, eidi=eidi: e.indirect_dma_start(out=G[:], out_offset=None, in_=UV[:, :],
                                                                                      in_offset=bass.IndirectOffsetOnAxis(ap=eidi[:, jx:jx + 1], axis=0)),
                          P.S(("g", jx % NG)), reads=[eidi.b, tab], writes=[G.b])
                for s_ in range(GS):
                    jx = step * GS + s_
                    G = Guv[jx % NG]
                    if s_ == GS - 1 and POOL_SHARE:
                        P.op("pool", lambda e, G=G, hn=hn: e.tensor_tensor(out=junkp[:], in0=G[:, 0:1024], in1=hn[:], op=ALU.mult), reads=[G.b, hn.b], writes=[junkp.b])
                        P.op("act", lambda e, jx=jx: e.activation(out=junkp[:], in_=junkp[:], func=AF.Copy, accum_out=u[:, jx:jx + 1]), reads=[junkp.b], writes=[junkp.b], nodep=[ub[step]])
                        continue
                    if False:
                        P.op("dve", lambda e, G=G, jx=jx, hn=hn: e.scalar_tensor_tensor(out=junk[:], in0=G[:, 0:1024], scalar=1.0, in1=hn[:], op0=ALU.mult, op1=ALU.mult,
                                                                                       accum_out=u[:, jx:jx + 1]),
                             reads=[G.b, hn.b], nodep=[ub2[step]])
                        if thunks:
                            P.replay(thunks[:per])
                            del thunks[:per]
                        continue
                    pb_ = prodb[jx % 2]
                    P.op("dve", lambda e, G=G, hnb=hnb, pb_=pb_: e.tensor_tensor(out=pb_[:], in0=G[:, 0:1024], in1=hnb[:], op=ALU.mult), reads=[G.b, hnb.b], writes=[pb_.b])
                    P.op("act", lambda e, pb_=pb_, jx=jx: e.activation(out=pb_[:], in_=pb_[:], func=AF.Copy, accum_out=u[:, jx:jx + 1]),
                         reads=[pb_.b], writes=([pb_.b, ub[step]] if s_ == 0 else [pb_.b]), nodep=([] if s_ == 0 else [ub[step]]))
                    if thunks:
                        P.replay(thunks[:per])
                        del thunks[:per]
            if 1 <= step <= NGRP:
                g = step - 1
                cs = slice(g * GS, (g + 1) * GS)
                P.op("dve", lambda e, cs=cs: e.tensor_mul(out=g1[:, cs], in0=u[:, cs], in1=u[:, cs]), reads=[ub[g], ub2[g]], writes=[g1b[g]])
                P.op("dve", lambda e, cs=cs: e.tensor_scalar(out=g1[:, cs], in0=g1[:, cs], scalar1=0.044715, scalar2=1.0, op0=ALU.mult, op1=ALU.add), reads=[g1b[g]], writes=[g1b[g]])
                P.op("dve", lambda e, cs=cs: e.tensor_mul(out=g1[:, cs], in0=g1[:, cs], in1=u[:, cs]), reads=[g1b[g], ub[g]], writes=[g1b[g]])
                P.op("dve", lambda e, cs=cs, gflat=gflat: e.tensor_mul(out=ug[:, cs], in0=u[:, cs], in1=gflat[:, cs]), reads=[ub[g], gate.b, ghb[g]], writes=[ghb[g]])
                P.op("act", lambda e, cs=cs: e.activation(out=g2[:, cs], in_=g1[:, cs], func=AF.Sigmoid, scale=1.5957691216057308), reads=[g1b[g]], writes=[g1b[g]])
        if thunks:
            P.replay(thunks)
        for n2 in range(2):
            P.op("dve", lambda e, n2=n2, acc=acc: e.tensor_add(out=acc[:, n2 * 512:(n2 + 1) * 512], in0=pV[n2][:], in1=acc[:, n2 * 512:(n2 + 1) * 512]),
                 reads=[pV[n2].b, acc.b], writes=[acc.b])
        rms(acc[:], acc.b, 1024)
        P.op("dve", lambda e, acc=acc: e.scalar_tensor_tensor(out=ot[:], in0=acc[:], scalar=rstd[:, 0:1], in1=fing[:], op0=ALU.mult, op1=ALU.mult),
             reads=[acc.b, rstd.b, fing.b], writes=[ot.b])
        P.dma("sp", lambda e, r0=r0: e.dma_start(out=C.out[r0:r0 + 128, :], in_=ot[:]), P.S("out"), reads=[ot.b], writes=[db("OUT", ti)])

    def record_stage1(ti):
        P.defer = []
        stage1(ti)
        th = P.defer
        P.defer = None
        return th

    NT = T // 128
    stage1(0)
    for ti in range(NT):
        th = record_stage1(ti + 1) if ti + 1 < NT else []
        gphase(ti, th)

    P.end_phase()
    es.close()
```
